# Optimizing a Trainium2 kernel written in Bass

```python
import math
import jax, jax.numpy as jnp
from jax import lax
import numpy as np

D_MODEL = 1024
BATCH = 4
SEQ = 4096
DEPTH = 2

N_A_LAYERS = DEPTH // 2
N_B_LAYERS = DEPTH - N_A_LAYERS
N_DENSE_LAYERS = (DEPTH + 1) // 2
N_MOE_LAYERS = DEPTH // 2

SB_HEADS = 16
SB_HEAD_DIM = D_MODEL // SB_HEADS
DIFF_HEADS = 8
DIFF_HEAD_DIM = D_MODEL // (2 * DIFF_HEADS)
DIFF_V_DIM = 2 * DIFF_HEAD_DIM
LAMBDA_INIT_STD = 0.1
ROPE_THETA = 500000.0
ROPE_DIM = DIFF_HEAD_DIM // 4
D_FF_DENSE = 2816
N_EXPERTS = 8
TOP_K = 2
D_FF_EXPERT = 3584

BLOCK = 128
RMS_EPS = 1e-5

kernel_name = "yoco_stickbreaking_diffattn_moe_trunk"


def rmsnorm(x, g):
    xf = x.astype(jnp.float32)
    y = xf * lax.rsqrt(jnp.mean(xf * xf, axis=-1, keepdims=True) + RMS_EPS)
    return (y * g.astype(jnp.float32)).astype(x.dtype)


def rope_tables(seq_len):
    inv_freq = ROPE_THETA ** (-jnp.arange(0, ROPE_DIM, 2, dtype=jnp.float32) / ROPE_DIM)
    ang = jnp.arange(seq_len, dtype=jnp.float32)[:, None] * inv_freq[None, :]
    ang = jnp.concatenate([ang, ang], axis=-1)
    return jnp.cos(ang), jnp.sin(ang)


def apply_partial_rope(x, cos, sin):
    xr = x[..., :ROPE_DIM].astype(jnp.float32)
    x1, x2 = xr[..., : ROPE_DIM // 2], xr[..., ROPE_DIM // 2:]
    rot = jnp.concatenate([-x2, x1], axis=-1)
    xr = xr * cos + rot * sin
    return jnp.concatenate([xr.astype(x.dtype), x[..., ROPE_DIM:]], axis=-1)


def stick_breaking_attention(q, k, v):
    seq_len, d = q.shape[2], q.shape[3]
    scale = d ** -0.5
    outs = []
    for i in range(seq_len // BLOCK):
        end = (i + 1) * BLOCK
        z = jnp.einsum('bhqd,bhkd->bhqk', q[:, :, i * BLOCK:end], k[:, :, :end]).astype(jnp.float32) * scale
        t_pos = i * BLOCK + jnp.arange(BLOCK)[:, None]
        s_pos = jnp.arange(end)[None, :]
        before = s_pos < t_pos
        log_keep = jnp.where(before, jax.nn.log_sigmoid(-z), 0.0)
        between = lax.cumsum(log_keep, axis=3, reverse=True) - log_keep
        w = jnp.where(before, jnp.exp(jax.nn.log_sigmoid(z) + between), 0.0)
        outs.append(jnp.einsum('bhqk,bhkd->bhqd', w.astype(v.dtype), v[:, :, :end]))
    return jnp.concatenate(outs, axis=2)


def differential_attention(q1, q2, k1, k2, v, lam):
    seq_len, d = q1.shape[2], q1.shape[3]
    scale = d ** -0.5
    outs = []
    for i in range(seq_len // BLOCK):
        end = (i + 1) * BLOCK
        t_pos = i * BLOCK + jnp.arange(BLOCK)[:, None]
        s_pos = jnp.arange(end)[None, :]
        causal = s_pos <= t_pos
        s1 = jnp.einsum('bhqd,bhkd->bhqk', q1[:, :, i * BLOCK:end], k1[:, :, :end]).astype(jnp.float32) * scale
        s2 = jnp.einsum('bhqd,bhkd->bhqk', q2[:, :, i * BLOCK:end], k2[:, :, :end]).astype(jnp.float32) * scale
        p1 = jax.nn.softmax(jnp.where(causal, s1, -jnp.inf), axis=-1)
        p2 = jax.nn.softmax(jnp.where(causal, s2, -jnp.inf), axis=-1)
        w = p1 - lam * p2
        outs.append(jnp.einsum('bhqk,bhkd->bhqd', w.astype(v.dtype), v[:, :, :end]))
    return jnp.concatenate(outs, axis=2)


def swiglu(x, w_gate_up, w_down):
    gu = x @ w_gate_up
    g, u = jnp.split(gu, 2, axis=-1)
    return (jax.nn.silu(g) * u) @ w_down


def moe_swiglu(x, w_router, w_gate_up, w_down):
    xt = x.reshape(-1, x.shape[-1])
    logits = (xt @ w_router).astype(jnp.float32)
    top_vals, top_idx = lax.top_k(logits, TOP_K)
    gates = jax.nn.softmax(top_vals, axis=-1)
    combine = jnp.sum(jax.nn.one_hot(top_idx, N_EXPERTS, dtype=jnp.float32) * gates[..., None], axis=1)
    out = jnp.zeros_like(xt)
    for e in range(N_EXPERTS):
        out = out + combine[:, e:e + 1].astype(xt.dtype) * swiglu(xt, w_gate_up[e], w_down[e])
    return out.reshape(x.shape)


def setup_inputs(seed: int = 0) -> dict:
    key = jax.random.key(seed)
    ks = jax.random.split(key, 24)
    f32 = jnp.float32
    D = D_MODEL

    def w(k, shape, fan_in):
        return jax.random.normal(k, shape, f32) * (fan_in ** -0.5)

    def gain(k, shape):
        return 1.0 + 0.02 * jax.random.normal(k, shape, f32)

    kv_width = 2 * DIFF_HEADS * DIFF_HEAD_DIM + DIFF_HEADS * DIFF_V_DIM
    return {
        "x": jax.random.normal(ks[0], (BATCH, SEQ, D), f32),
        "sb_norm_g": gain(ks[1], (N_A_LAYERS, D)),
        "sb_w_qkv": w(ks[2], (N_A_LAYERS, D, 3 * SB_HEADS * SB_HEAD_DIM), D),
        "sb_w_o": w(ks[3], (N_A_LAYERS, SB_HEADS * SB_HEAD_DIM, D), SB_HEADS * SB_HEAD_DIM),
        "kv_norm_g": gain(ks[4], (D,)),
        "diff_w_kv": w(ks[5], (D, kv_width), D),
        "diff_norm_g": gain(ks[6], (N_B_LAYERS, D)),
        "diff_w_q": w(ks[7], (N_B_LAYERS, D, 2 * DIFF_HEADS * DIFF_HEAD_DIM), D),
        "diff_lambda_q1": LAMBDA_INIT_STD * jax.random.normal(ks[8], (N_B_LAYERS, DIFF_HEAD_DIM), f32),
        "diff_lambda_k1": LAMBDA_INIT_STD * jax.random.normal(ks[9], (N_B_LAYERS, DIFF_HEAD_DIM), f32),
        "diff_lambda_q2": LAMBDA_INIT_STD * jax.random.normal(ks[10], (N_B_LAYERS, DIFF_HEAD_DIM), f32),
        "diff_lambda_k2": LAMBDA_INIT_STD * jax.random.normal(ks[11], (N_B_LAYERS, DIFF_HEAD_DIM), f32),
        "diff_subln_g": gain(ks[12], (N_B_LAYERS, DIFF_V_DIM)),
        "diff_w_o": w(ks[13], (N_B_LAYERS, DIFF_HEADS * DIFF_V_DIM, D), DIFF_HEADS * DIFF_V_DIM),
        "ffn_norm_g": gain(ks[14], (DEPTH, D)),
        "dense_w_gate_up": w(ks[15], (N_DENSE_LAYERS, D, 2 * D_FF_DENSE), D),
        "dense_w_down": w(ks[16], (N_DENSE_LAYERS, D_FF_DENSE, D), D_FF_DENSE),
        "moe_w_router": w(ks[17], (N_MOE_LAYERS, D, N_EXPERTS), D),
        "moe_w_gate_up": w(ks[18], (N_MOE_LAYERS, N_EXPERTS, D, 2 * D_FF_EXPERT), D),
        "moe_w_down": w(ks[19], (N_MOE_LAYERS, N_EXPERTS, D_FF_EXPERT, D), D_FF_EXPERT),
        "final_norm_g": gain(ks[20], (D,)),
    }


def reference(x, sb_norm_g, sb_w_qkv, sb_w_o, kv_norm_g, diff_w_kv, diff_norm_g, diff_w_q,
              diff_lambda_q1, diff_lambda_k1, diff_lambda_q2, diff_lambda_k2, diff_subln_g, diff_w_o,
              ffn_norm_g, dense_w_gate_up, dense_w_down, moe_w_router, moe_w_gate_up, moe_w_down,
              final_norm_g):
    B, S, D = x.shape
    cos, sin = rope_tables(S)
    h = x
    k1 = k2 = v_shared = None
    for l in range(DEPTH):
        if l == N_A_LAYERS:
            kv = rmsnorm(h, kv_norm_g) @ diff_w_kv
            k_part = kv[..., : 2 * DIFF_HEADS * DIFF_HEAD_DIM].reshape(B, S, DIFF_HEADS, 2, DIFF_HEAD_DIM)
            k_part = apply_partial_rope(jnp.transpose(k_part, (3, 0, 2, 1, 4)), cos, sin)
            k1, k2 = k_part[0], k_part[1]
            v_shared = jnp.transpose(
                kv[..., 2 * DIFF_HEADS * DIFF_HEAD_DIM:].reshape(B, S, DIFF_HEADS, DIFF_V_DIM), (0, 2, 1, 3))
        if l < N_A_LAYERS:
            a = l
            qkv = rmsnorm(h, sb_norm_g[a]) @ sb_w_qkv[a]
            qkv = jnp.transpose(qkv.reshape(B, S, 3, SB_HEADS, SB_HEAD_DIM), (2, 0, 3, 1, 4))
            o = stick_breaking_attention(qkv[0], qkv[1], qkv[2])
            o = jnp.transpose(o, (0, 2, 1, 3)).reshape(B, S, SB_HEADS * SB_HEAD_DIM)
            h = h + o @ sb_w_o[a]
        else:
            b = l - N_A_LAYERS
            lambda_init = 0.8 - 0.6 * math.exp(-0.3 * l)
            lam = (jnp.exp(jnp.sum(diff_lambda_q1[b].astype(jnp.float32) * diff_lambda_k1[b].astype(jnp.float32)))
                   - jnp.exp(jnp.sum(diff_lambda_q2[b].astype(jnp.float32) * diff_lambda_k2[b].astype(jnp.float32)))
                   + lambda_init)
            q = (rmsnorm(h, diff_norm_g[b]) @ diff_w_q[b]).reshape(B, S, DIFF_HEADS, 2, DIFF_HEAD_DIM)
            q = apply_partial_rope(jnp.transpose(q, (3, 0, 2, 1, 4)), cos, sin)
            o = differential_attention(q[0], q[1], k1, k2, v_shared, lam)
            o = rmsnorm(o, diff_subln_g[b]) * (1.0 - lambda_init)
            o = jnp.transpose(o, (0, 2, 1, 3)).reshape(B, S, DIFF_HEADS * DIFF_V_DIM)
            h = h + o @ diff_w_o[b]
        hn = rmsnorm(h, ffn_norm_g[l])
        if l % 2 == 0:
            h = h + swiglu(hn, dense_w_gate_up[l // 2], dense_w_down[l // 2])
        else:
            i = l // 2
            h = h + moe_swiglu(hn, moe_w_router[i], moe_w_gate_up[i], moe_w_down[i])
    return rmsnorm(h, final_norm_g)
```

```python
import contextlib
import math
import numpy as np
import concourse.bass as bass
import concourse.mybir as mybir
from concourse.bass_utils import run_bass_kernel_spmd

F32 = mybir.dt.float32
BF16 = mybir.dt.bfloat16
AF = mybir.ActivationFunctionType
ALU = mybir.AluOpType
AX = mybir.AxisListType

D = 1024
EPS = 1e-5
F_DENSE = 2816
F_EXP = 3584
N_EXP = 8
ROPE_THETA = 500000.0
ROPE_DIM = 16
PE, ACT, DVE, POOL, SP = "pe", "act", "dve", "pool", "sp"
ENGS = (PE, ACT, DVE, POOL, SP)


class Res:
    __slots__ = ("name", "lw", "rd", "track")

    def __init__(self, name, track=True):
        self.name = name
        self.lw = None
        self.rd = []
        self.track = track


class DSem:
    __slots__ = ("h", "count", "last")

    def __init__(self, h):
        self.h = h
        self.count = 0
        self.last = None


class Op:
    __slots__ = ("eng", "fn", "waits", "inc", "cnt", "dsem", "dval")

    def __init__(self, eng, fn, dsem):
        self.eng = eng
        self.fn = fn
        self.waits = []
        self.inc = False
        self.cnt = 0
        self.dsem = dsem
        self.dval = 0


class Prog:
    def __init__(self, nc, es):
        self.nc = nc
        self.es = es
        self.ops = {e: [] for e in ENGS}
        self.esem = {e: es.enter_context(nc.semaphore("sem_" + e)) for e in (PE, ACT, DVE, POOL)}
        self.dsems = []
        self.last_real = {e: None for e in ENGS}
        self.qsems = {q: [self.dsem("dq_%s_%d" % (q, i)) for i in range(10)] for q in (SP, POOL)}
        self.qctr = {SP: 0, POOL: 0}

    def dsem(self, name):
        d = DSem(self.es.enter_context(self.nc.semaphore(name)))
        self.dsems.append(d)
        return d

    def add(self, eng, fn, reads=(), writes=(), dsem=None):
        deps = []
        if dsem == "auto":
            pool = self.qsems[eng]
            dsem = pool[self.qctr[eng] % len(pool)]
            self.qctr[eng] += 1
            if dsem.last is not None:
                deps.append(dsem.last)
        op = Op(eng, fn, dsem)
        for r in reads:
            if r.lw is not None:
                deps.append(r.lw)
        for w in writes:
            if w.lw is not None:
                deps.append(w.lw)
            deps.extend(w.rd)
        seen = set()
        for d in deps:
            if d is op or id(d) in seen:
                continue
            seen.add(id(d))
            if d.dsem is None and d.eng == PE and eng == PE:
                continue
            op.waits.append(d)
            if d.dsem is None:
                d.inc = True
        for r in reads:
            if r.track:
                r.rd.append(op)
        for w in writes:
            w.lw = op
            w.rd = []
        if dsem is not None:
            dsem.count += 16
            op.dval = dsem.count
            dsem.last = op
        self.ops[eng].append(op)
        if fn is not None:
            self.last_real[eng] = op
        return op

    def barrier(self):
        lasts = [self.last_real[e] for e in (PE, ACT, DVE, POOL) if self.last_real[e] is not None
                 and self.last_real[e].dsem is None]
        dl = [d.last for d in self.dsems if d.last is not None]
        for e in ENGS:
            op = Op(e, None, None)
            for d in lasts + dl:
                if d.dsem is None and d.eng == e and e == PE:
                    continue
                op.waits.append(d)
                if d.dsem is None:
                    d.inc = True
            self.ops[e].append(op)

    def emit(self):
        nc = self.nc
        for e in (PE, ACT, DVE, POOL):
            c = 0
            for op in self.ops[e]:
                if op.dsem is None and op.inc:
                    c += 1
                    op.cnt = c
        with nc.Block() as block:
            regs = {PE: block.tensor, ACT: block.scalar, DVE: block.vector, POOL: block.gpsimd, SP: block.sync}
            for e in ENGS:
                def body(engobj, e=e):
                    seen = {}
                    for op in self.ops[e]:
                        for d in op.waits:
                            if d.dsem is not None:
                                key, h, val = id(d.dsem), d.dsem.h, d.dval
                            else:
                                key, h, val = d.eng, self.esem[d.eng], d.cnt
                            if seen.get(key, 0) >= val:
                                continue
                            seen[key] = val
                            engobj.wait_ge(h, val)
                        if op.fn is None:
                            continue
                        ins = op.fn(engobj)
                        if op.dsem is not None:
                            ins.then_inc(op.dsem.h, 16)
                        elif op.inc:
                            ins.then_inc(self.esem[e], 1)
                regs[e](body)


class Arena:
    def __init__(self, base, words):
        self.base = base
        self.words = words
        self.off = 0

    def alloc(self, cols, dt):
        nb = 4 if dt == F32 else 2
        w = (cols * nb + 3) // 4
        assert self.off + w <= self.words, ("SBUF arena overflow", self.off, w, self.words)
        a = self.base[:, self.off:self.off + w]
        self.off += w
        return a if dt == F32 else a.bitcast(dt)


def tile_map(S):
    nt = S // 256
    g = [[0] * nt, [0] * nt]
    for m in range(nt):
        k, r = divmod(m, 2)
        g[0][m] = 4 * k + (0 if r == 0 else 3)
        g[1][m] = 4 * k + (1 if r == 0 else 2)
    return g


C_ID, C_TRI, C_OMT, C_ONE, C_MS, C_MC, C_RT = range(7)


def make_consts():
    c = np.zeros((128, 7, 128), np.float32)
    i = np.arange(128)
    c[:, C_ID] = np.eye(128, dtype=np.float32)
    c[:, C_TRI] = (i[:, None] >= i[None, :])
    c[:, C_OMT] = (i[:, None] < i[None, :])
    c[:, C_ONE] = 1.0
    c[:, C_MS] = (i[:, None] < i[None, :])
    c[:, C_MC] = (i[:, None] <= i[None, :])
    rt = np.zeros((128, 128), np.float32)
    for base in (0, 64):
        for d in range(8):
            rt[base + d + 8, base + d] = -1.0
            rt[base + d, base + d + 8] = 1.0
    c[:, C_RT] = rt
    return c.reshape(128, 7 * 128)


def rope_tables(pos):
    inv = (ROPE_THETA ** (-np.arange(0, ROPE_DIM, 2, dtype=np.float32) / ROPE_DIM)).astype(np.float32)
    ang = pos.astype(np.float32)[:, None] * inv[None, :]
    ang = np.concatenate([ang, ang], axis=-1)
    cos = np.ones((64, len(pos)), np.float32)
    sin = np.zeros((64, len(pos)), np.float32)
    cos[:ROPE_DIM] = np.cos(ang).T
    sin[:ROPE_DIM] = np.sin(ang).T
    return np.stack([np.concatenate([cos, cos], 0), np.concatenate([sin, sin], 0)], 0)


class Builder:
    def __init__(self, S, debug=(), stop_after=None):
        self.S = S
        self.NTS = S // 128
        self.NT = self.NTS // 2
        self.TOK = self.NT * 128
        self.debug = set(debug)
        self.stop_after = stop_after
        self.nc = bass.Bass("TRN2", target_bir_lowering=False)
        self.resd = {}

    def din(self, name, shape, dt=F32):
        return self.nc.dram_tensor(name, list(shape), dt, kind="ExternalInput").ap()

    def dscr(self, name, shape, dt):
        kind = "ExternalOutput" if name in self.debug else "Internal"
        return self.nc.dram_tensor(name, list(shape), dt, kind=kind).ap()

    def R(self, *key, track=True):
        r = self.resd.get(key)
        if r is None:
            r = Res(key, track)
            self.resd[key] = r
        return r

    def newR(self, name, track=True):
        return Res(name, track)

    def mm(self, out, lhsT, rhs, start, stop, reads, writes):
        return self.P.add(PE, lambda e: e.matmul(out, lhsT, rhs, start=start, stop=stop), reads, writes)

    def tr(self, out, in_, ident, reads, writes):
        return self.P.add(PE, lambda e: e.transpose(out, in_, ident), reads, writes)

    def act(self, out, in_, func, reads, writes, bias=None, scale=None, accum=None):
        kw = {}
        if bias is not None:
            kw["bias"] = bias
        if scale is not None:
            kw["scale"] = scale
        if accum is not None:
            kw["accum_out"] = accum
        return self.P.add(ACT, lambda e: e.activation(out=out, in_=in_, func=func, **kw), reads, writes)

    def tt(self, out, a, b, op, reads, writes, eng=DVE):
        return self.P.add(eng, lambda e: e.tensor_tensor(out=out, in0=a, in1=b, op=op), reads, writes)

    def ts(self, out, a, s1, s2, op0, op1, reads, writes, eng=DVE):
        if op1 is None:
            return self.P.add(eng, lambda e: e.tensor_scalar(out=out, in0=a, scalar1=s1, scalar2=None, op0=op0),
                              reads, writes)
        return self.P.add(eng, lambda e: e.tensor_scalar(out=out, in0=a, scalar1=s1, scalar2=s2, op0=op0, op1=op1),
                          reads, writes)

    def stt(self, out, a, scalar, b, op0, op1, reads, writes):
        return self.P.add(DVE, lambda e: e.scalar_tensor_tensor(out=out, in0=a, scalar=scalar, in1=b, op0=op0, op1=op1),
                          reads, writes)

    def cp(self, out, in_, reads, writes, eng=DVE):
        return self.P.add(eng, lambda e: e.tensor_copy(out=out, in_=in_), reads, writes)

    def dma(self, q, out, in_, dsem, reads, writes):
        return self.P.add(q, lambda e: e.dma_start(out=out, in_=in_), reads, writes, dsem="auto")

    def build(self):
        nc = self.nc
        S, NTS, NT, TOK = self.S, self.NTS, self.NT, self.TOK
        I = {}
        I["x"] = self.din("x", [S, D])
        I["cst"] = self.din("cst", [128, 7 * 128])
        I["ropek"] = self.din("ropek", [2, 128, S])
        I["ropeq"] = self.din("ropeq", [2, 128, TOK])
        I["sel"] = self.din("sel", [128, 2])
        I["mask1"] = self.din("mask1", [NT, 2, 128, 128])
        I["sb_norm_g"] = self.din("sb_norm_g", [D])
        I["sb_w_qkv"] = self.din("sb_w_qkv", [D, 3 * D])
        I["sb_w_o"] = self.din("sb_w_o", [D, D])
        I["kv_norm_g"] = self.din("kv_norm_g", [D])
        I["diff_w_kv"] = self.din("diff_w_kv", [D, 2 * D])
        I["diff_norm_g"] = self.din("diff_norm_g", [D])
        I["diff_w_q"] = self.din("diff_w_q", [D, D])
        I["lam"] = self.din("lam", [4, 64])
        I["diff_subln_g"] = self.din("diff_subln_g", [128, 1])
        I["diff_w_o"] = self.din("diff_w_o", [D, D])
        I["ffn_norm_g"] = self.din("ffn_norm_g", [2, D])
        I["dense_w_gate_up"] = self.din("dense_w_gate_up", [D, 2 * F_DENSE])
        I["dense_w_down"] = self.din("dense_w_down", [F_DENSE, D])
        I["moe_w_router"] = self.din("moe_w_router", [D, N_EXP])
        if self.stop_after in (None, "G"):
            I["moe_w_gate_up"] = self.din("moe_w_gate_up", [N_EXP, D, 2 * F_EXP])
            I["moe_w_down"] = self.din("moe_w_down", [N_EXP, F_EXP, D])
        I["final_norm_g"] = self.din("final_norm_g", [D])
        self.I = I
        out = nc.dram_tensor("out", [TOK, D], F32, kind="ExternalOutput").ap()
        X = {}
        X["q0T"] = self.dscr("q0T", [D, S], BF16)
        X["k0T"] = self.dscr("k0T", [D, S], BF16)
        X["v0"] = self.dscr("v0", [S, D], BF16)
        X["a0T"] = self.dscr("a0T", [D, S], BF16)
        X["h1"] = self.dscr("h1", [S, D], F32)
        X["k1T"] = self.dscr("k1T", [D, S], BF16)
        X["v1"] = self.dscr("v1", [S, D], BF16)
        X["hown"] = self.dscr("hown", [TOK, D], F32)
        X["q1T"] = self.dscr("q1T", [D, TOK], BF16)
        X["a1T"] = self.dscr("a1T", [D, TOK], BF16)
        self.X = X

        with contextlib.ExitStack() as es:
            self.P = Prog(nc, es)
            arena_words = 51 * 1024
            arena_t = es.enter_context(nc.sbuf_tensor("arena", [128, arena_words], F32))
            self.arena = Arena(arena_t[:, :], arena_words)
            ps_t = es.enter_context(nc.psum_tensor("ps", [128, 8, 512], F32))
            self.ps = ps_t
            self.psR = [self.newR(("psum", b)) for b in range(8)]
            self.sem_ld = [None] * 12
            self.sem_w = [None] * 8
            self.sem_st = [None] * 6
            self.sem_c = None
            self.sem_cp = None
            self.setup_consts()
            self.const_mark = self.arena.off

            stages = [
                ("A", self.stage_A), ("B", self.stage_B), ("C", self.stage_C), ("D", self.stage_D),
                ("E", self.stage_E), ("F", self.stage_F), ("G", lambda: self.stage_G(out)),
            ]
            for name, fn in stages:
                self.arena.off = self.const_mark
                fn()
                self.P.barrier()
                if self.stop_after == name:
                    break
            self.P.barrier()
            self.P.emit()
        return nc

    def setup_consts(self):
        A = self.arena
        cb = A.alloc(7 * 128, BF16).rearrange("p (a b) -> p a b", a=7)
        cf = A.alloc(7 * 128, F32).rearrange("p (a b) -> p a b", a=7)
        self.cR = self.newR("consts", track=False)
        src = self.I["cst"].rearrange("p (a b) -> p a b", a=7)
        self.dma(POOL, cb, src, self.sem_cp, [], [self.cR])
        self.dma(SP, cf, src, self.sem_c, [], [self.cR])
        self.cb, self.cf = cb, cf
        self.sel_sb = A.alloc(2, F32)
        self.eps_t = A.alloc(1, F32)
        self.P.add(DVE, lambda e: e.memset(self.eps_t, EPS), [], [self.cR])
        self.dma(SP, self.sel_sb, self.I["sel"], self.sem_c, [], [self.cR])

    def gain_bcast(self, gap, q=SP):
        t = self.arena.alloc(D, F32)
        r = self.newR("gain", track=False)
        self.dma(q, t, gap.unsqueeze(0).broadcast_to([128, D]), self.sem_c, [], [r])
        return t, r

    def rmsnorm_tile(self, src, srcR, gB, gR, out, outR, junk, junkR, stat, statR):
        self.P.add(DVE, lambda e: e.memset(stat[:, 0:1], 0.0), [], [statR])
        self.act(junk, src, AF.Square, [srcR, statR], [junkR, statR], accum=stat[:, 0:1])
        self.act(stat[:, 1:2], stat[:, 0:1], AF.Sqrt, [statR, self.cR], [statR], bias=self.eps_t, scale=1.0 / D)
        self.P.add(DVE, lambda e: e.reciprocal(out=stat[:, 1:2], in_=stat[:, 1:2]), [statR], [statR])
        self.stt(out, src, stat[:, 1:2], gB, ALU.mult, ALU.mult, [srcR, statR, gR], [outR])

    def proj_phase(self, tag, ntiles, src_fn, gain_ap, W_ap, ncols, fm_outs, tm_outs):
        A, P = self.arena, self.P
        cR = self.cR
        W_sb = A.alloc(8 * ncols, BF16).rearrange("p (k n) -> p k n", k=8)
        WR = self.newR(tag + "W", track=False)
        Wv = W_ap.rearrange("(k p) n -> p k n", p=128)
        for k in range(8):
            self.dma(POOL, W_sb[:, k, :], Wv[:, k, :], self.sem_w[k % 4], [], [WR])
        gB, gR = self.gain_bcast(gain_ap)
        GT = min(4, ntiles)
        NG = ntiles // GT
        GW = GT * 128
        xs = [A.alloc(D, F32) for _ in range(3)]
        xsR = [self.newR(tag + "xs%d" % i) for i in range(3)]
        xs2 = A.alloc(D, F32)
        xs2R = self.newR(tag + "xs2")
        xn = [A.alloc(D, BF16) for _ in range(2)]
        xnR = [self.newR(tag + "xn%d" % i) for i in range(2)]
        junk = A.alloc(D, BF16)
        junkR = self.newR(tag + "junk")
        stat = [A.alloc(2, F32) for _ in range(2)]
        statR = [self.newR(tag + "stat%d" % i) for i in range(2)]
        xnT = [A.alloc(8 * GW, BF16).rearrange("p (k t) -> p k t", k=8) for _ in range(2)]
        xnTR = [self.newR(tag + "xnT%d" % i) for i in range(2)]
        stg = [A.alloc(512, BF16) for _ in range(3)]
        stgR = [self.newR(tag + "stg%d" % i) for i in range(3)]
        need_rope = any(o.get("rope") is not None for o in fm_outs)
        if need_rope:
            qf = [A.alloc(GW, F32) for _ in range(2)]
            qfR = [self.newR(tag + "qf%d" % i) for i in range(2)]
            t1 = A.alloc(GW, F32)
            t1R = self.newR(tag + "t1")
            rt = [A.alloc(2 * GW, F32).rearrange("p (a t) -> p a t", a=2) for _ in range(2)]
            rtR = [self.newR(tag + "rt%d" % i) for i in range(2)]
        ps = self.ps
        n_ld = 0
        n_acc = 0
        n_stg = 0
        n_tr = 0
        n_qf = 0
        for g in range(NG):
            xT = xnT[g % 2]
            xTR = xnTR[g % 2]
            for tl in range(GT):
                ti = g * GT + tl
                srcs = src_fn(ti)
                s0 = xs[n_ld % 3]
                s0R = xsR[n_ld % 3]
                ld = self.sem_ld[n_ld % 3]
                n_ld += 1
                self.dma(SP, s0, srcs[0][0], ld, [], [s0R])
                if len(srcs) == 2:
                    self.dma(SP, xs2, srcs[1][0], self.sem_ld[3], [], [xs2R])
                    self.ts(s0, s0, self.sel_sb[:, 0:1], None, ALU.mult, None, [s0R, cR], [s0R])
                    self.stt(s0, xs2, self.sel_sb[:, 1:2], s0, ALU.mult, ALU.add, [xs2R, s0R, cR], [s0R])
                    if srcs[0][1] is not None:
                        self.dma(SP, srcs[0][1], s0, self.sem_st[0], [s0R], [self.R("hown", ti)])
                xo = xn[ti % 2]
                xoR = xnR[ti % 2]
                self.rmsnorm_tile(s0, s0R, gB, gR, xo, xoR, junk, junkR, stat[ti % 2], statR[ti % 2])
                bank = 6 + (n_tr % 2)
                n_tr += 1
                pb = ps[:, bank, :].bitcast(BF16).rearrange("p (k t) -> p k t", k=8)
                for k in range(8):
                    self.tr(pb[:, k, :], xo[:, k * 128:(k + 1) * 128], self.cb[:, C_ID, :], [xoR, cR], [self.psR[bank]])
                self.P.add(ACT, lambda e, o=xT[:, :, tl * 128:(tl + 1) * 128], i=pb: e.copy(out=o, in_=i),
                           [self.psR[bank]], [xTR])
            for o in fm_outs:
                if o.get("rope") is not None:
                    r_t = rt[g % 2]
                    r_R = rtR[g % 2]
                    self.dma(SP, r_t, o["rope"][:, :, g * GW:(g + 1) * GW].rearrange("a p t -> p a t"),
                             self.sem_ld[4 + g % 2], [], [r_R])
                for fo in range(8):
                    bank = n_acc % 3
                    n_acc += 1
                    pa = ps[:, bank, 0:GW]
                    for k in range(8):
                        self.mm(pa, W_sb[:, k, o["col0"] + fo * 128:o["col0"] + (fo + 1) * 128], xT[:, k, :],
                                k == 0, k == 7, [WR, xTR], [self.psR[bank]])
                    sg = stg[n_stg % 3][:, 0:GW]
                    sgR = stgR[n_stg % 3]
                    n_stg += 1
                    if o.get("rope") is None:
                        self.act(sg, pa, AF.Copy, [self.psR[bank]], [sgR], scale=float(o.get("scale", 1.0)))
                    else:
                        qq = qf[n_qf % 2]
                        qqR = qfR[n_qf % 2]
                        n_qf += 1
                        self.act(qq, pa, AF.Copy, [self.psR[bank]], [qqR], scale=float(o.get("scale", 1.0)))
                        rb = 3 + (n_qf % 2)
                        pr = ps[:, rb, 0:GW]
                        self.mm(pr, self.cf[:, C_RT, :], qq, True, True, [cR, qqR], [self.psR[rb]])
                        self.tt(t1, qq, r_t[:, 0, :], ALU.mult, [qqR, r_R], [t1R])
                        self.tt(qq, pr, r_t[:, 1, :], ALU.mult, [self.psR[rb], r_R], [qqR])
                        self.tt(sg, t1, qq, ALU.add, [t1R, qqR], [sgR])
                    self.dma(POOL, o["dst"][fo * 128:(fo + 1) * 128, g * GW:(g + 1) * GW], sg,
                             self.sem_st[1], [sgR], [self.R(o["name"], "g", g)])
            for o in tm_outs:
                for tl in range(GT):
                    ti = g * GT + tl
                    for hf in range(2):
                        bank = n_acc % 3
                        n_acc += 1
                        pa = ps[:, bank, :]
                        for k in range(8):
                            self.mm(pa, xT[:, k, tl * 128:(tl + 1) * 128],
                                    W_sb[:, k, o["col0"] + hf * 512:o["col0"] + (hf + 1) * 512],
                                    k == 0, k == 7, [WR, xTR], [self.psR[bank]])
                        sg = stg[n_stg % 3]
                        sgR = stgR[n_stg % 3]
                        n_stg += 1
                        self.cp(sg, pa, [self.psR[bank]], [sgR])
                        self.dma(POOL, o["dst"][ti * 128:(ti + 1) * 128, hf * 512:(hf + 1) * 512], sg,
                                 self.sem_st[2], [sgR], [self.R(o["name"], "t", ti)])

    def stage_A(self):
        I, X = self.I, self.X
        self.proj_phase(
            "A", self.NTS, lambda ti: [(I["x"][ti * 128:(ti + 1) * 128, :], None)], I["sb_norm_g"],
            I["sb_w_qkv"], 3 * D,
            fm_outs=[dict(name="q0T", col0=0, scale=0.125, dst=X["q0T"]),
                     dict(name="k0T", col0=D, dst=X["k0T"])],
            tm_outs=[dict(name="v0", col0=2 * D, dst=X["v0"])])

    def stage_B(self):
        A, P, ps, cR = self.arena, self.P, self.ps, self.cR
        S, NTS = self.S, self.NTS
        X = self.X
        cb = self.cb
        kT = A.alloc(2 * S, BF16).rearrange("p (c t) -> p c t", c=2)
        qT = A.alloc(2 * NTS * 256, BF16).rearrange("p (c t w) -> p c t w", c=2, t=NTS)
        vv = A.alloc(NTS * 256, BF16).rearrange("p (b d) -> p b d", b=NTS)
        kR, qR, vR = self.newR("B_k"), self.newR("B_q"), self.newR("B_v")
        self.P.add(DVE, lambda e: e.memset(qT, 0.0), [], [qR])
        NB = 3
        e_sb = [A.alloc(512, F32) for _ in range(NB)]
        eR = [self.newR("B_e%d" % i) for i in range(NB)]
        sp_sb = [A.alloc(512, BF16) for _ in range(NB)]
        spR = [self.newR("B_sp%d" % i) for i in range(NB)]
        x_sb = [A.alloc(512, F32) for _ in range(2)]
        xR = [self.newR("B_x%d" % i) for i in range(2)]
        w_sb = [A.alloc(512, BF16) for _ in range(2)]
        wR = [self.newR("B_w%d" % i) for i in range(2)]
        o_sb = [A.alloc(512, BF16) for _ in range(2)]
        oR = [self.newR("B_o%d" % i) for i in range(2)]
        ss_sb = [A.alloc(512, BF16) for _ in range(2)]
        ssR = [self.newR("B_ss%d" % i) for i in range(2)]
        maskS4 = cb[:, C_MS, :].unsqueeze(1).broadcast_to([128, 4, 128])
        all_src = [self.R("q0T", "g", g) for g in range(max(1, NTS // 4))] + \
                  [self.R("k0T", "g", g) for g in range(max(1, NTS // 4))] + \
                  [self.R("v0", "t", t) for t in range(NTS)]
        n_step = 0
        n_q = 0
        for hg in range(4):
            for c in range(2):
                self.dma(SP, kT[:, c, :], X["k0T"][(hg * 2 + c) * 128:(hg * 2 + c + 1) * 128, :], self.sem_ld[0],
                         all_src, [kR])
                r0 = (hg * 2 + c) * 128
                for t0 in range(0, NTS, 8):
                    t1 = min(NTS, t0 + 8)
                    self.dma(SP, qT[0:64, c, t0:t1, 0:128],
                             X["q0T"][r0:r0 + 64, t0 * 128:t1 * 128].rearrange("p (t w) -> p t w", w=128),
                             None, all_src, [qR])
                    self.dma(SP, qT[64:128, c, t0:t1, 128:256],
                             X["q0T"][r0 + 64:r0 + 128, t0 * 128:t1 * 128].rearrange("p (t w) -> p t w", w=128),
                             None, all_src, [qR])
            for b0 in range(0, NTS, 4):
                b1 = min(NTS, b0 + 4)
                self.dma(SP, vv[:, b0:b1, :],
                         X["v0"][b0 * 128:b1 * 128, hg * 256:(hg + 1) * 256].rearrange("(b p) d -> p b d", p=128),
                         None, all_src, [vR])
            for qi in range(NTS):
                ob0 = 4 + 2 * (n_q % 2)
                n_q += 1
                steps = list(range(qi, -1, -1))

                LV = getattr(self, "dbg_level", 5)

                def stage1(kb, idx):
                    if LV < 2:
                        return
                    zb = idx % 2
                    z = ps[:, zb, :]
                    for cc in range(2):
                        self.mm(z[:, cc * 256:(cc + 1) * 256], kT[:, cc, kb * 128:(kb + 1) * 128],
                                qT[:, cc, qi, :], True, True, [kR, qR], [self.psR[zb]])
                    b = idx % NB
                    self.act(e_sb[b], z, AF.Exp, [self.psR[zb]], [eR[b]])
                    self.act(sp_sb[b], e_sb[b], AF.Ln, [eR[b]], [spR[b]], bias=1.0)
                    if kb == qi:
                        self.tt(sp_sb[b].rearrange("p (h t) -> p h t", h=4),
                                sp_sb[b].rearrange("p (h t) -> p h t", h=4), maskS4, ALU.mult,
                                [spR[b], cR], [spR[b]])

                def stage2(kb, idx, i):
                    if LV < 3:
                        return
                    b = idx % NB
                    b2 = idx % 2
                    first = (kb == qi)
                    last = (kb == 0)
                    pbank = 2 + (idx % 2)
                    Pp = ps[:, pbank, :]
                    if i == 0:
                        self.mm(Pp, cb[:, C_TRI, :], sp_sb[b], True, True, [cR, spR[b]], [self.psR[pbank]])
                    else:
                        if i == 1:
                            car, carR = sp_sb[(idx - 1) % NB], spR[(idx - 1) % NB]
                        else:
                            car, carR = ss_sb[i % 2], ssR[i % 2]
                        self.mm(Pp, cb[:, C_TRI, :], sp_sb[b], True, False, [cR, spR[b]], [self.psR[pbank]])
                        self.mm(Pp, cb[:, C_ONE, :], car, False, True, [cR, carR], [self.psR[pbank]])
                        if not last:
                            self.tt(ss_sb[(i + 1) % 2], car, sp_sb[b], ALU.add, [carR, spR[b]], [ssR[(i + 1) % 2]])
                    self.act(x_sb[b2], Pp, AF.Exp, [self.psR[pbank]], [xR[b2]], scale=-1.0)
                    if LV < 4:
                        return
                    self.tt(w_sb[b2], e_sb[b], x_sb[b2], ALU.mult, [eR[b], xR[b2]], [wR[b2]])
                    if first:
                        self.tt(w_sb[b2].rearrange("p (h t) -> p h t", h=4),
                                w_sb[b2].rearrange("p (h t) -> p h t", h=4), maskS4, ALU.mult,
                                [wR[b2], cR], [wR[b2]])
                    for pr in range(2):
                        self.mm(ps[:, ob0 + pr, 0:256], vv[:, kb, pr * 128:(pr + 1) * 128],
                                w_sb[b2][:, pr * 256:(pr + 1) * 256], first, last, [vR, wR[b2]],
                                [self.psR[ob0 + pr]])

                stage1(steps[0], n_step)
                for i, kb in enumerate(steps):
                    if i + 1 < len(steps):
                        stage1(steps[i + 1], n_step + i + 1)
                    stage2(kb, n_step + i, i)
                n_step += len(steps)
                if LV < 5:
                    continue
                ob = o_sb[qi % 2]
                self.cp(ob.rearrange("p (r c) -> p r c", r=2), ps[:, ob0:ob0 + 2, 0:256],
                        [self.psR[ob0], self.psR[ob0 + 1]], [oR[qi % 2]])
                for pr in range(2):
                    for hf in range(2):
                        r0 = hg * 256 + pr * 128 + hf * 64
                        self.dma(POOL, X["a0T"][r0:r0 + 64, qi * 128:(qi + 1) * 128],
                                 ob[hf * 64:(hf + 1) * 64, pr * 256 + hf * 128:pr * 256 + (hf + 1) * 128], None,
                                 [oR[qi % 2]], [self.R("a0T", qi // 4)])

    def tail_phase(self, tag, ntiles, aT, aT_res, Wo_ap, resid_fn, resid_res, gain_ap, experts, router_ap,
                   final_gain_ap, dst, dst_name):
        A, P, ps, cR = self.arena, self.P, self.ps, self.cR
        cb, cf = self.cb, self.cf
        GT = min(4, ntiles)
        NG = ntiles // GT
        GW = GT * 128
        Wo = A.alloc(8 * D, BF16).rearrange("p (k n) -> p k n", k=8)
        WoR = self.newR(tag + "Wo", track=False)
        Wov = Wo_ap.rearrange("(k p) n -> p k n", p=128)
        for k in range(8):
            self.dma(POOL, Wo[:, k, :], Wov[:, k, :], self.sem_w[4 + k % 2], [], [WoR])
        gB, gR = self.gain_bcast(gain_ap)
        if final_gain_ap is not None:
            gF, gFR = self.gain_bcast(final_gain_ap)
        if router_ap is not None:
            wr = A.alloc(8 * N_EXP, F32).rearrange("p (k n) -> p k n", k=8)
            wrR = self.newR(tag + "wr", track=False)
            self.dma(SP, wr, router_ap.rearrange("(k p) n -> p k n", p=128), self.sem_c, [], [wrR])
            hnT32 = A.alloc(8 * 128, F32).rearrange("p (k t) -> p k t", k=8)
            hnT32R = self.newR(tag + "hnT32")
            comb = A.alloc(GT * N_EXP, F32).rearrange("p (t n) -> p t n", t=GT)
            combR = self.newR(tag + "comb")
            rw = A.alloc(64, F32)
            rwR = self.newR(tag + "rw")
        aT_sb = A.alloc(8 * GW, BF16).rearrange("p (k t) -> p k t", k=8)
        aTR = self.newR(tag + "aT")
        acc = A.alloc(GT * D, F32).rearrange("p (t n) -> p t n", t=GT)
        accR = [self.newR(tag + "acc%d" % i) for i in range(GT)]
        rs = [A.alloc(D, F32) for _ in range(2)]
        rsR = [self.newR(tag + "rs%d" % i) for i in range(2)]
        hn = A.alloc(D, F32 if router_ap is not None else BF16)
        hnR = self.newR(tag + "hn")
        junk = A.alloc(D, BF16)
        junkR = self.newR(tag + "junk")
        stat = A.alloc(2, F32)
        statR = self.newR(tag + "stat")
        hnT = A.alloc(8 * GW, BF16).rearrange("p (k t) -> p k t", k=8)
        hnTR = self.newR(tag + "hnT")
        NFmax = max(F for (_, _, F) in experts) // 128
        actT = A.alloc(NFmax * GW, BF16).rearrange("p (f t) -> p f t", f=NFmax)
        actR = self.newR(tag + "actT")
        sg = [A.alloc(GW, F32) for _ in range(2)]
        sgR = [self.newR(tag + "sg%d" % i) for i in range(2)]
        NWS = 3
        wgu = [A.alloc(8 * 2 * 512, BF16).rearrange("p (k a n) -> p k a n", k=8, a=2) for _ in range(NWS)]
        wguR = [self.newR(tag + "wgu%d" % i) for i in range(NWS)]
        NDS = 2
        NBDmax = max((7 if (F // 128) % 7 == 0 else 11) for (_, _, F) in experts)
        wd = [A.alloc(NBDmax * D, BF16).rearrange("p (c n) -> p c n", c=NBDmax) for _ in range(NDS)]
        wdR = [self.newR(tag + "wd%d" % i) for i in range(NDS)]
        ot, otR = rs, rsR
        n_acc = 0
        n_gu = 0
        n_wgu = 0
        n_wd = 0
        n_sg = 0
        for g in range(NG):
            self.dma(SP, aT_sb, aT[:, g * GW:(g + 1) * GW].rearrange("(k p) t -> p k t", p=128), self.sem_ld[6],
                     aT_res(g), [aTR])
            for tl in range(GT):
                ti = g * GT + tl
                r_t, r_R = rs[ti % 2], rsR[ti % 2]
                self.dma(SP, r_t, resid_fn(ti), self.sem_ld[7 + ti % 2], resid_res(ti), [r_R])
                for hf in range(2):
                    bank = n_acc % 2
                    n_acc += 1
                    pa = ps[:, bank, :]
                    for k in range(8):
                        self.mm(pa, aT_sb[:, k, tl * 128:(tl + 1) * 128], Wo[:, k, hf * 512:(hf + 1) * 512],
                                k == 0, k == 7, [aTR, WoR], [self.psR[bank]])
                    self.tt(acc[:, tl, hf * 512:(hf + 1) * 512], pa, r_t[:, hf * 512:(hf + 1) * 512], ALU.add,
                            [self.psR[bank], r_R], [accR[tl]])
                self.rmsnorm_tile(acc[:, tl, :], accR[tl], gB, gR, hn, hnR, junk, junkR, stat, statR)
                if router_ap is None:
                    bank = 6 + (ti % 2)
                    pb = ps[:, bank, :].bitcast(BF16).rearrange("p (k t) -> p k t", k=8)
                    for k in range(8):
                        self.tr(pb[:, k, :], hn[:, k * 128:(k + 1) * 128], cb[:, C_ID, :], [hnR, cR], [self.psR[bank]])
                    self.P.add(ACT, lambda e, o=hnT[:, :, tl * 128:(tl + 1) * 128], i=pb: e.copy(out=o, in_=i),
                               [self.psR[bank]], [hnTR])
                else:
                    p32 = ps[:, 6:8, :].rearrange("p b (k t) -> p (b k) t", k=4)
                    for k in range(8):
                        self.tr(p32[:, k, :], hn[:, k * 128:(k + 1) * 128], cf[:, C_ID, :], [hnR, cR],
                                [self.psR[6], self.psR[7]])
                    self.P.add(ACT, lambda e, o=hnT32, i=p32: e.copy(out=o, in_=i), [self.psR[6], self.psR[7]], [hnT32R])
                    self.cp(hnT[:, :, tl * 128:(tl + 1) * 128], hnT32, [hnT32R], [hnTR])
                    bank = n_acc % 2
                    n_acc += 1
                    pl = ps[:, bank, 0:N_EXP]
                    for k in range(8):
                        self.mm(pl, hnT32[:, k, :], wr[:, k, :], k == 0, k == 7, [hnT32R, wrR], [self.psR[bank]])
                    L, E1, L2, E2, SC, C1 = (rw[:, i * 8:(i + 1) * 8] for i in range(6))
                    rr = [rwR]
                    self.cp(L, pl, [self.psR[bank]], rr)
                    P.add(DVE, lambda e, o=SC[:, 0:1], i=L: e.tensor_reduce(out=o, in_=i, axis=AX.X, op=ALU.max), rr, rr)
                    self.ts(E1, L, SC[:, 0:1], None, ALU.is_equal, None, rr, rr)
                    self.stt(L2, E1, -1e30, L, ALU.mult, ALU.add, rr, rr)
                    P.add(DVE, lambda e, o=SC[:, 1:2], i=L2: e.tensor_reduce(out=o, in_=i, axis=AX.X, op=ALU.max), rr, rr)
                    self.ts(E2, L2, SC[:, 1:2], None, ALU.is_equal, None, rr, rr)
                    self.tt(SC[:, 2:3], SC[:, 1:2], SC[:, 0:1], ALU.subtract, rr, rr)
                    self.act(SC[:, 3:4], SC[:, 2:3], AF.Exp, rr, rr)
                    self.ts(SC[:, 4:5], SC[:, 3:4], 1.0, None, ALU.add, None, rr, rr)
                    P.add(DVE, lambda e, o=SC[:, 5:6], i=SC[:, 4:5]: e.reciprocal(out=o, in_=i), rr, rr)
                    self.tt(SC[:, 6:7], SC[:, 3:4], SC[:, 5:6], ALU.mult, rr, rr)
                    self.ts(C1, E1, SC[:, 5:6], None, ALU.mult, None, rr, rr)
                    self.stt(comb[:, tl, :], E2, SC[:, 6:7], C1, ALU.mult, ALU.add, rr, [combR])
            for ei, (Wgu_ap, Wd_ap, F) in enumerate(experts):
                NF = F // 128
                BF = 4 if NF % 4 == 0 else 2
                Wg_v = Wgu_ap.rearrange("(k p) n -> p k n", p=128)
                for blk in range(NF // BF):
                    s = n_wgu % NWS
                    n_wgu += 1
                    wsl, wslR = wgu[s], wguR[s]
                    bw = BF * 128
                    self.dma(POOL, wsl[:, :, 0, 0:bw], Wg_v[:, :, blk * bw:(blk + 1) * bw], self.sem_w[s], [], [wslR])
                    self.dma(POOL, wsl[:, :, 1, 0:bw], Wg_v[:, :, F + blk * bw:F + (blk + 1) * bw], self.sem_w[s], [],
                             [wslR])
                    for c in range(BF):
                        fc = blk * BF + c
                        gb = 2 + (n_gu % 2)
                        ub = 4 + (n_gu % 2)
                        n_gu += 1
                        pg = ps[:, gb, 0:GW]
                        pu = ps[:, ub, 0:GW]
                        for k in range(8):
                            self.mm(pg, wsl[:, k, 0, c * 128:(c + 1) * 128], hnT[:, k, :], k == 0, k == 7,
                                    [wslR, hnTR], [self.psR[gb]])
                        for k in range(8):
                            self.mm(pu, wsl[:, k, 1, c * 128:(c + 1) * 128], hnT[:, k, :], k == 0, k == 7,
                                    [wslR, hnTR], [self.psR[ub]])
                        s_t, s_R = sg[n_sg % 2], sgR[n_sg % 2]
                        n_sg += 1
                        self.act(s_t, pg, AF.Silu, [self.psR[gb]], [s_R])
                        self.tt(actT[:, fc, :], s_t, pu, ALU.mult, [s_R, self.psR[ub]], [actR])
                NBD = 7 if NF % 7 == 0 else 11
                Wd_v = Wd_ap.rearrange("(c p) n -> p c n", p=128)
                for blk in range(NF // NBD):
                    s = n_wd % NDS
                    n_wd += 1
                    dsl, dslR = wd[s], wdR[s]
                    self.dma(POOL, dsl[:, 0:NBD, :], Wd_v[:, blk * NBD:(blk + 1) * NBD, :], self.sem_w[4 + s], [], [dslR])
                    for tl in range(GT):
                        for hf in range(2):
                            bank = n_acc % 2
                            n_acc += 1
                            pa = ps[:, bank, :]
                            for c in range(NBD):
                                self.mm(pa, actT[:, blk * NBD + c, tl * 128:(tl + 1) * 128],
                                        dsl[:, c, hf * 512:(hf + 1) * 512], c == 0, c == NBD - 1,
                                        [actR, dslR], [self.psR[bank]])
                            av = acc[:, tl, hf * 512:(hf + 1) * 512]
                            if router_ap is None:
                                self.tt(av, pa, av, ALU.add, [self.psR[bank], accR[tl]], [accR[tl]])
                            else:
                                self.stt(av, pa, comb[:, tl, ei:ei + 1], av, ALU.mult, ALU.add,
                                         [self.psR[bank], accR[tl], combR], [accR[tl]])
            for tl in range(GT):
                ti = g * GT + tl
                if final_gain_ap is None:
                    self.dma(SP, dst[ti * 128:(ti + 1) * 128, :], acc[:, tl, :], self.sem_st[4], [accR[tl]],
                             [self.R(dst_name, ti)])
                else:
                    o_t, o_R = ot[ti % 2], otR[ti % 2]
                    self.rmsnorm_tile(acc[:, tl, :], accR[tl], gF, gFR, o_t, o_R, junk, junkR, stat, statR)
                    self.dma(SP, dst[ti * 128:(ti + 1) * 128, :], o_t, self.sem_st[4], [o_R], [self.R(dst_name, ti)])

    def stage_C(self):
        I, X = self.I, self.X
        self.tail_phase(
            "C", self.NTS, X["a0T"], lambda g: [self.R("a0T", g)], I["sb_w_o"],
            lambda ti: I["x"][ti * 128:(ti + 1) * 128, :], lambda ti: [],
            I["ffn_norm_g"][0], [(I["dense_w_gate_up"], I["dense_w_down"], F_DENSE)], None, None,
            X["h1"], "h1")

    def stage_D(self):
        I, X = self.I, self.X
        self.proj_phase(
            "D", self.NTS, lambda ti: [(X["h1"][ti * 128:(ti + 1) * 128, :], None)], I["kv_norm_g"],
            I["diff_w_kv"], 2 * D,
            fm_outs=[dict(name="k1T", col0=0, dst=X["k1T"], rope=I["ropek"])],
            tm_outs=[dict(name="v1", col0=D, dst=X["v1"])])

    def stage_E(self):
        I, X = self.I, self.X
        gm = tile_map(self.S)

        def src(m):
            a, b = gm[0][m], gm[1][m]
            return [(X["h1"][a * 128:(a + 1) * 128, :], X["hown"][m * 128:(m + 1) * 128, :]),
                    (X["h1"][b * 128:(b + 1) * 128, :], None)]

        self.proj_phase(
            "E", self.NT, src, I["diff_norm_g"], I["diff_w_q"], D,
            fm_outs=[dict(name="q1T", col0=0, dst=X["q1T"], rope=I["ropeq"], scale=0.125)], tm_outs=[])

    def stage_F(self):
        A, P, ps, cR = self.arena, self.P, self.ps, self.cR
        S, NTS, NT, TOK = self.S, self.NTS, self.NT, self.TOK
        I, X = self.I, self.X
        cb, cf = self.cb, self.cf
        lv = A.alloc(4 * 64, F32).rearrange("p (a d) -> p a d", a=4)
        lw = A.alloc(8, F32)
        lR = self.newR("F_lam")
        self.dma(SP, lv, I["lam"].unsqueeze(0).broadcast_to([128, 4, 64]), self.sem_c, [], [lR])
        pr = A.alloc(128, F32).rearrange("p (a d) -> p a d", a=2)
        self.tt(pr[:, 0, :], lv[:, 0, :], lv[:, 1, :], ALU.mult, [lR], [lR])
        self.tt(pr[:, 1, :], lv[:, 2, :], lv[:, 3, :], ALU.mult, [lR], [lR])
        P.add(DVE, lambda e: e.tensor_reduce(out=lw[:, 0:2], in_=pr, axis=AX.X, op=ALU.add), [lR], [lR])
        self.act(lw[:, 2:4], lw[:, 0:2], AF.Exp, [lR], [lR])
        lam_init = 0.8 - 0.6 * math.exp(-0.3 * 1)
        self.tt(lw[:, 4:5], lw[:, 3:4], lw[:, 2:3], ALU.subtract, [lR], [lR])
        self.ts(lw[:, 5:6], lw[:, 4:5], -float(lam_init), None, ALU.add, None, [lR], [lR])
        neg_lam = lw[:, 5:6]
        gs = A.alloc(2, F32)
        self.dma(SP, gs[:, 0:1], I["diff_subln_g"], self.sem_c, [], [lR])
        self.ts(gs[:, 1:2], gs[:, 0:1], float(1.0 - lam_init), None, ALU.mult, None, [lR], [lR])
        gsub = gs[:, 1:2]
        kT = A.alloc(2 * S, BF16).rearrange("p (c t) -> p c t", c=2)
        qT = A.alloc(2 * NT * 256, BF16).rearrange("p (c t w) -> p c t w", c=2, t=NT)
        vv = A.alloc(NTS * 256, BF16).rearrange("p (b d) -> p b d", b=NTS)
        kR, qR, vR = self.newR("F_k"), self.newR("F_q"), self.newR("F_v")
        self.P.add(DVE, lambda e: e.memset(qT, 0.0), [], [qR])
        mk = A.alloc(NT * 2 * 128, BF16).rearrange("p (m a t) -> p m a t", m=NT, a=2)
        mkR = self.newR("F_mask", track=False)
        for m0 in range(0, NT, 2):
            m1 = min(NT, m0 + 2)
            self.dma(POOL, mk[:, m0:m1], I["mask1"][m0:m1].rearrange("m a s t -> s m a t"), None, [], [mkR])
        e_sb = [A.alloc(512, BF16) for _ in range(3)]
        eR = [self.newR("F_e%d" % i) for i in range(3)]
        f1 = A.alloc(512, F32)
        f2 = A.alloc(512, F32)
        f3 = A.alloc(256, F32)
        f4 = A.alloc(256, F32)
        fR = self.newR("F_fin")
        on = [A.alloc(256, BF16) for _ in range(2)]
        onR = [self.newR("F_on%d" % i) for i in range(2)]
        src_all = [self.R("k1T", "g", g) for g in range(max(1, NTS // 4))] + \
                  [self.R("v1", "t", t) for t in range(NTS)] + \
                  [self.R("q1T", "g", g) for g in range(max(1, NT // 4))]
        n_step = 0
        n_m = 0
        for hp in range(4):
            for c in range(2):
                h = hp * 2 + c
                self.dma(SP, kT[:, c, :], X["k1T"][h * 128:(h + 1) * 128, :], self.sem_ld[0], src_all, [kR])
                for t0 in range(0, NT, 8):
                    t1 = min(NT, t0 + 8)
                    self.dma(SP, qT[0:64, c, t0:t1, 0:128],
                             X["q1T"][h * 128:h * 128 + 64, t0 * 128:t1 * 128].rearrange("p (t w) -> p t w", w=128),
                             None, src_all, [qR])
                    self.dma(SP, qT[64:128, c, t0:t1, 128:256],
                             X["q1T"][h * 128 + 64:(h + 1) * 128, t0 * 128:t1 * 128].rearrange("p (t w) -> p t w", w=128),
                             None, src_all, [qR])
            for b0 in range(0, NTS, 4):
                b1 = min(NTS, b0 + 4)
                self.dma(SP, vv[:, b0:b1, :],
                         X["v1"][b0 * 128:b1 * 128, hp * 256:(hp + 1) * 256].rearrange("(b p) d -> p b d", p=128),
                         None, src_all, [vR])
            for m in range(NT):
                k4, r = divmod(m, 2)
                gmax = 4 * k4 + (1 if r == 0 else 3)
                ob0 = 2 + 2 * (n_m % 2)
                sbank = 6 + (n_m % 2)
                n_m += 1
                Oa = ps[:, ob0:ob0 + 2, 0:256]
                Sa = ps[:, sbank, :]
                for kb in range(gmax + 1):
                    zb = n_step % 2
                    eb = n_step % 3
                    n_step += 1
                    z = ps[:, zb, :]
                    for hh in range(2):
                        self.mm(z[:, hh * 256:(hh + 1) * 256], kT[:, hh, kb * 128:(kb + 1) * 128],
                                qT[:, hh, m, :], True, True, [kR, qR], [self.psR[zb]])
                    self.act(e_sb[eb], z, AF.Exp, [self.psR[zb]], [eR[eb]])
                    if kb >= gmax - 1:
                        mm_ = mk[:, m, kb - (gmax - 1), :].unsqueeze(1).broadcast_to([128, 4, 128])
                        ev = e_sb[eb].rearrange("p (h t) -> p h t", h=4)
                        self.tt(ev, ev, mm_, ALU.mult, [eR[eb], mkR], [eR[eb]])
                    for hh in range(2):
                        self.mm(ps[:, ob0 + hh, 0:256], vv[:, kb, hh * 128:(hh + 1) * 128],
                                e_sb[eb][:, hh * 256:(hh + 1) * 256], kb == 0, kb == gmax, [vR, eR[eb]],
                                [self.psR[ob0 + hh]])
                    self.mm(Sa, cb[:, C_ONE, :], e_sb[eb], kb == 0, kb == gmax, [cR, eR[eb]], [self.psR[sbank]])
                rr = [fR]
                self.act(f1, Sa, AF.Ln, [self.psR[sbank]], rr)
                self.act(f1, f1, AF.Exp, rr, rr, scale=-1.0)
                self.tt(f2.rearrange("p (h c) -> p h c", h=2), Oa, f1.rearrange("p (h c) -> p h c", h=2), ALU.mult,
                        [self.psR[ob0], self.psR[ob0 + 1]] + rr, rr)
                f2v = f2.rearrange("p (h a t) -> p h a t", h=2, a=2)
                f3v = f3.rearrange("p (h t) -> p h t", h=2)
                self.stt(f3v, f2v[:, :, 1, :], neg_lam, f2v[:, :, 0, :], ALU.mult, ALU.add, rr + [lR], rr)
                self.tt(f4, f3, f3, ALU.mult, rr, rr)
                mb = n_step % 2
                pm = ps[:, mb, 0:256]
                self.mm(pm, cf[:, C_ONE, :], f4, True, True, [cR] + rr, [self.psR[mb]])
                self.act(f4, pm, AF.Ln, [self.psR[mb], cR], rr, bias=self.eps_t, scale=1.0 / 128.0)
                self.act(f4, f4, AF.Exp, rr, rr, scale=-0.5)
                self.tt(f3, f3, f4, ALU.mult, rr, rr)
                o_t, o_R = on[m % 2], onR[m % 2]
                self.ts(o_t, f3, gsub, None, ALU.mult, None, rr + [lR], [o_R])
                dst = X["a1T"][hp * 256:(hp + 1) * 256, m * 128:(m + 1) * 128].rearrange("(h d) t -> d h t", d=128)
                self.dma(POOL, dst, o_t.rearrange("p (h t) -> p h t", h=2), self.sem_st[3], [o_R],
                         [self.R("a1T", m // 4)])

    def stage_G(self, out):
        I, X = self.I, self.X
        experts = [(I["moe_w_gate_up"][e], I["moe_w_down"][e], F_EXP) for e in range(N_EXP)]
        self.tail_phase(
            "G", self.NT, X["a1T"], lambda g: [self.R("a1T", g)], I["diff_w_o"],
            lambda ti: X["hown"][ti * 128:(ti + 1) * 128, :], lambda ti: [self.R("hown", ti)],
            I["ffn_norm_g"][1], experts, I["moe_w_router"], I["final_norm_g"], out, "out")


def make_in_maps(inputs, S):
    gm = tile_map(S)
    NT = S // 256
    cst = make_consts()
    ropek = rope_tables(np.arange(S))
    i = np.arange(128)
    ones = np.ones((128, 128), np.float32)
    zeros = np.zeros((128, 128), np.float32)
    diag = (i[:, None] <= i[None, :]).astype(np.float32)
    f = lambda a: np.ascontiguousarray(np.asarray(a, dtype=np.float32))
    shared = {
        "cst": cst, "ropek": ropek,
        "sb_norm_g": f(inputs["sb_norm_g"][0]), "sb_w_qkv": f(inputs["sb_w_qkv"][0]), "sb_w_o": f(inputs["sb_w_o"][0]),
        "kv_norm_g": f(inputs["kv_norm_g"]), "diff_w_kv": f(inputs["diff_w_kv"]),
        "diff_norm_g": f(inputs["diff_norm_g"][0]), "diff_w_q": f(inputs["diff_w_q"][0]),
        "lam": f(np.stack([inputs["diff_lambda_q1"][0], inputs["diff_lambda_k1"][0],
                           inputs["diff_lambda_q2"][0], inputs["diff_lambda_k2"][0]], 0)),
        "diff_subln_g": f(inputs["diff_subln_g"][0]).reshape(128, 1),
        "diff_w_o": f(inputs["diff_w_o"][0]), "ffn_norm_g": f(inputs["ffn_norm_g"]),
        "dense_w_gate_up": f(inputs["dense_w_gate_up"][0]), "dense_w_down": f(inputs["dense_w_down"][0]),
        "moe_w_router": f(inputs["moe_w_router"][0]), "moe_w_gate_up": f(inputs["moe_w_gate_up"][0]),
        "moe_w_down": f(inputs["moe_w_down"][0]), "final_norm_g": f(inputs["final_norm_g"]),
    }
    x = np.asarray(inputs["x"], dtype=np.float32)
    maps = []
    for c in range(8):
        b, j = divmod(c, 2)
        pos = np.concatenate([gm[j][m] * 128 + np.arange(128) for m in range(NT)])
        mask1 = np.zeros((NT, 2, 128, 128), np.float32)
        for m in range(NT):
            k4, r = divmod(m, 2)
            gmax = 4 * k4 + (1 if r == 0 else 3)
            g = gm[j][m]
            for a, kb in enumerate((gmax - 1, gmax)):
                mask1[m, a] = ones if kb < g else (diag if kb == g else zeros)
        sel = np.zeros((128, 2), np.float32)
        sel[:, j] = 1.0
        d = dict(shared)
        d.update({"x": np.ascontiguousarray(x[b]), "ropeq": rope_tables(pos), "sel": sel, "mask1": mask1})
        maps.append(d)
    return maps


_NC_CACHE = {}


def kernel(**inputs):
    S = int(np.asarray(inputs["x"]).shape[1])
    B = int(np.asarray(inputs["x"]).shape[0])
    assert B == 4
    if S not in _NC_CACHE:
        _NC_CACHE[S] = Builder(S).build()
    nc = _NC_CACHE[S]
    maps = make_in_maps(inputs, S)
    res = run_bass_kernel_spmd(nc, maps, core_ids=list(range(8)))
    gm = tile_map(S)
    NT = S // 256
    out = np.zeros((B, S, D), np.float32)
    for c in range(8):
        b, j = divmod(c, 2)
        o = np.asarray(res.results[c]["out"]).reshape(NT, 128, D)
        for m in range(NT):
            g = gm[j][m]
            out[b, g * 128:(g + 1) * 128] = o[m]
    return out
```

```python
import contextlib
import math
import numpy as np
import concourse.bass as bass
import concourse.mybir as mybir
from concourse.bass_utils import run_bass_kernel_spmd

F32 = mybir.dt.float32
BF16 = mybir.dt.bfloat16
AF = mybir.ActivationFunctionType
ALU = mybir.AluOpType
AX = mybir.AxisListType

D = 1024
EPS = 1e-5
F_DENSE = 2816
F_EXP = 3584
N_EXP = 8
ROPE_THETA = 500000.0
ROPE_DIM = 16
PE, ACT, DVE, POOL, SP = "pe", "act", "dve", "pool", "sp"
ENGS = (PE, ACT, DVE, POOL, SP)


class Res:
    __slots__ = ("name", "lw", "rd", "track")

    def __init__(self, name, track=True):
        self.name = name
        self.lw = None
        self.rd = []
        self.track = track


class DSem:
    __slots__ = ("h", "count", "last")

    def __init__(self, h):
        self.h = h
        self.count = 0
        self.last = None


class Op:
    __slots__ = ("eng", "fn", "waits", "inc", "cnt", "dsem", "dval")

    def __init__(self, eng, fn, dsem):
        self.eng = eng
        self.fn = fn
        self.waits = []
        self.inc = False
        self.cnt = 0
        self.dsem = dsem
        self.dval = 0


class Prog:
    def __init__(self, nc, es):
        self.nc = nc
        self.es = es
        self.ops = {e: [] for e in ENGS}
        self.esem = {e: es.enter_context(nc.semaphore("sem_" + e)) for e in (PE, ACT, DVE, POOL)}
        self.dsems = []
        self.last_real = {e: None for e in ENGS}
        self.qsems = {q: [self.dsem("dq_%s_%d" % (q, i)) for i in range(10)] for q in (SP, POOL)}
        self.qctr = {SP: 0, POOL: 0}

    def dsem(self, name):
        d = DSem(self.es.enter_context(self.nc.semaphore(name)))
        self.dsems.append(d)
        return d

    def add(self, eng, fn, reads=(), writes=(), dsem=None):
        deps = []
        if dsem == "auto":
            pool = self.qsems[eng]
            dsem = pool[self.qctr[eng] % len(pool)]
            self.qctr[eng] += 1
            if dsem.last is not None:
                deps.append(dsem.last)
        op = Op(eng, fn, dsem)
        for r in reads:
            if r.lw is not None:
                deps.append(r.lw)
        for w in writes:
            if w.lw is not None:
                deps.append(w.lw)
            deps.extend(w.rd)
        seen = set()
        for d in deps:
            if d is op or id(d) in seen:
                continue
            seen.add(id(d))
            if d.dsem is None and d.eng == PE and eng == PE:
                continue
            op.waits.append(d)
            if d.dsem is None:
                d.inc = True
        for r in reads:
            if r.track:
                r.rd.append(op)
        for w in writes:
            w.lw = op
            w.rd = []
        if dsem is not None:
            dsem.count += 16
            op.dval = dsem.count
            dsem.last = op
        self.ops[eng].append(op)
        if fn is not None:
            self.last_real[eng] = op
        return op

    def barrier(self):
        lasts = [self.last_real[e] for e in (PE, ACT, DVE, POOL) if self.last_real[e] is not None
                 and self.last_real[e].dsem is None]
        dl = [d.last for d in self.dsems if d.last is not None]
        for e in ENGS:
            op = Op(e, None, None)
            for d in lasts + dl:
                if d.dsem is None and d.eng == e and e == PE:
                    continue
                op.waits.append(d)
                if d.dsem is None:
                    d.inc = True
            self.ops[e].append(op)

    def emit(self):
        nc = self.nc
        for e in (PE, ACT, DVE, POOL):
            c = 0
            for op in self.ops[e]:
                if op.dsem is None and op.inc:
                    c += 1
                    op.cnt = c
        with nc.Block() as block:
            regs = {PE: block.tensor, ACT: block.scalar, DVE: block.vector, POOL: block.gpsimd, SP: block.sync}
            for e in ENGS:
                def body(engobj, e=e):
                    seen = {}
                    for op in self.ops[e]:
                        for d in op.waits:
                            if d.dsem is not None:
                                key, h, val = id(d.dsem), d.dsem.h, d.dval
                            else:
                                key, h, val = d.eng, self.esem[d.eng], d.cnt
                            if seen.get(key, 0) >= val:
                                continue
                            seen[key] = val
                            engobj.wait_ge(h, val)
                        if op.fn is None:
                            continue
                        ins = op.fn(engobj)
                        if op.dsem is not None:
                            ins.then_inc(op.dsem.h, 16)
                        elif op.inc:
                            ins.then_inc(self.esem[e], 1)
                regs[e](body)


class Arena:
    def __init__(self, base, words):
        self.base = base
        self.words = words
        self.off = 0

    def alloc(self, cols, dt):
        nb = 4 if dt == F32 else 2
        w = (cols * nb + 3) // 4
        assert self.off + w <= self.words, ("SBUF arena overflow", self.off, w, self.words)
        a = self.base[:, self.off:self.off + w]
        self.off += w
        return a if dt == F32 else a.bitcast(dt)


def tile_map(S):
    nt = S // 256
    g = [[0] * nt, [0] * nt]
    for m in range(nt):
        k, r = divmod(m, 2)
        g[0][m] = 4 * k + (0 if r == 0 else 3)
        g[1][m] = 4 * k + (1 if r == 0 else 2)
    return g


C_ID, C_TRI, C_OMT, C_ONE, C_MS, C_MC, C_RT = range(7)


def make_consts():
    c = np.zeros((128, 7, 128), np.float32)
    i = np.arange(128)
    c[:, C_ID] = np.eye(128, dtype=np.float32)
    c[:, C_TRI] = (i[:, None] >= i[None, :])
    c[:, C_OMT] = (i[:, None] < i[None, :])
    c[:, C_ONE] = 1.0
    c[:, C_MS] = (i[:, None] < i[None, :])
    c[:, C_MC] = (i[:, None] <= i[None, :])
    rt = np.zeros((128, 128), np.float32)
    for base in (0, 64):
        for d in range(8):
            rt[base + d + 8, base + d] = -1.0
            rt[base + d, base + d + 8] = 1.0
    c[:, C_RT] = rt
    return c.reshape(128, 7 * 128)


def rope_tables(pos):
    inv = (ROPE_THETA ** (-np.arange(0, ROPE_DIM, 2, dtype=np.float32) / ROPE_DIM)).astype(np.float32)
    ang = pos.astype(np.float32)[:, None] * inv[None, :]
    ang = np.concatenate([ang, ang], axis=-1)
    cos = np.ones((64, len(pos)), np.float32)
    sin = np.zeros((64, len(pos)), np.float32)
    cos[:ROPE_DIM] = np.cos(ang).T
    sin[:ROPE_DIM] = np.sin(ang).T
    return np.stack([np.concatenate([cos, cos], 0), np.concatenate([sin, sin], 0)], 0)


class Builder:
    def __init__(self, S, debug=(), stop_after=None):
        self.S = S
        self.NTS = S // 128
        self.NT = self.NTS // 2
        self.TOK = self.NT * 128
        self.debug = set(debug)
        self.stop_after = stop_after
        self.nc = bass.Bass("TRN2", target_bir_lowering=False)
        self.resd = {}

    def din(self, name, shape, dt=F32):
        return self.nc.dram_tensor(name, list(shape), dt, kind="ExternalInput").ap()

    def dscr(self, name, shape, dt):
        kind = "ExternalOutput" if name in self.debug else "Internal"
        return self.nc.dram_tensor(name, list(shape), dt, kind=kind).ap()

    def R(self, *key, track=True):
        r = self.resd.get(key)
        if r is None:
            r = Res(key, track)
            self.resd[key] = r
        return r

    def newR(self, name, track=True):
        return Res(name, track)

    def mm(self, out, lhsT, rhs, start, stop, reads, writes):
        return self.P.add(PE, lambda e: e.matmul(out, lhsT, rhs, start=start, stop=stop), reads, writes)

    def tr(self, out, in_, ident, reads, writes):
        return self.P.add(PE, lambda e: e.transpose(out, in_, ident), reads, writes)

    def act(self, out, in_, func, reads, writes, bias=None, scale=None, accum=None):
        kw = {}
        if bias is not None:
            kw["bias"] = bias
        if scale is not None:
            kw["scale"] = scale
        if accum is not None:
            kw["accum_out"] = accum
        return self.P.add(ACT, lambda e: e.activation(out=out, in_=in_, func=func, **kw), reads, writes)

    def tt(self, out, a, b, op, reads, writes, eng=DVE):
        return self.P.add(eng, lambda e: e.tensor_tensor(out=out, in0=a, in1=b, op=op), reads, writes)

    def ts(self, out, a, s1, s2, op0, op1, reads, writes, eng=DVE):
        if op1 is None:
            return self.P.add(eng, lambda e: e.tensor_scalar(out=out, in0=a, scalar1=s1, scalar2=None, op0=op0),
                              reads, writes)
        return self.P.add(eng, lambda e: e.tensor_scalar(out=out, in0=a, scalar1=s1, scalar2=s2, op0=op0, op1=op1),
                          reads, writes)

    def stt(self, out, a, scalar, b, op0, op1, reads, writes):
        return self.P.add(DVE, lambda e: e.scalar_tensor_tensor(out=out, in0=a, scalar=scalar, in1=b, op0=op0, op1=op1),
                          reads, writes)

    def cp(self, out, in_, reads, writes, eng=DVE):
        return self.P.add(eng, lambda e: e.tensor_copy(out=out, in_=in_), reads, writes)

    def dma(self, q, out, in_, dsem, reads, writes):
        return self.P.add(q, lambda e: e.dma_start(out=out, in_=in_), reads, writes, dsem="auto")

    def build(self):
        nc = self.nc
        S, NTS, NT, TOK = self.S, self.NTS, self.NT, self.TOK
        I = {}
        I["x"] = self.din("x", [S, D])
        I["cst"] = self.din("cst", [128, 7 * 128])
        I["ropek"] = self.din("ropek", [2, 128, S])
        I["ropeq"] = self.din("ropeq", [2, 128, TOK])
        I["sel"] = self.din("sel", [128, 2])
        I["mask1"] = self.din("mask1", [NT, 2, 128, 128])
        I["sb_norm_g"] = self.din("sb_norm_g", [D])
        I["sb_w_qkv"] = self.din("sb_w_qkv", [D, 3 * D])
        I["sb_w_o"] = self.din("sb_w_o", [D, D])
        I["kv_norm_g"] = self.din("kv_norm_g", [D])
        I["diff_w_kv"] = self.din("diff_w_kv", [D, 2 * D])
        I["diff_norm_g"] = self.din("diff_norm_g", [D])
        I["diff_w_q"] = self.din("diff_w_q", [D, D])
        I["lam"] = self.din("lam", [4, 64])
        I["diff_subln_g"] = self.din("diff_subln_g", [128, 1])
        I["diff_w_o"] = self.din("diff_w_o", [D, D])
        I["ffn_norm_g"] = self.din("ffn_norm_g", [2, D])
        I["dense_w_gate_up"] = self.din("dense_w_gate_up", [D, 2 * F_DENSE])
        I["dense_w_down"] = self.din("dense_w_down", [F_DENSE, D])
        I["moe_w_router"] = self.din("moe_w_router", [D, N_EXP])
        if self.stop_after in (None, "G"):
            I["moe_w_gate_up"] = self.din("moe_w_gate_up", [N_EXP, D, 2 * F_EXP])
            I["moe_w_down"] = self.din("moe_w_down", [N_EXP, F_EXP, D])
        I["final_norm_g"] = self.din("final_norm_g", [D])
        self.I = I
        out = nc.dram_tensor("out", [TOK, D], F32, kind="ExternalOutput").ap()
        X = {}
        X["q0T"] = self.dscr("q0T", [D, S], BF16)
        X["k0T"] = self.dscr("k0T", [D, S], BF16)
        X["v0"] = self.dscr("v0", [S, D], BF16)
        X["a0T"] = self.dscr("a0T", [D, S], BF16)
        X["h1"] = self.dscr("h1", [S, D], F32)
        X["k1T"] = self.dscr("k1T", [D, S], BF16)
        X["v1"] = self.dscr("v1", [S, D], BF16)
        X["hown"] = self.dscr("hown", [TOK, D], F32)
        X["q1T"] = self.dscr("q1T", [D, TOK], BF16)
        X["a1T"] = self.dscr("a1T", [D, TOK], BF16)
        self.X = X

        with contextlib.ExitStack() as es:
            self.P = Prog(nc, es)
            arena_words = 51 * 1024
            arena_t = es.enter_context(nc.sbuf_tensor("arena", [128, arena_words], F32))
            self.arena = Arena(arena_t[:, :], arena_words)
            ps_t = es.enter_context(nc.psum_tensor("ps", [128, 8, 512], F32))
            self.ps = ps_t
            self.psR = [self.newR(("psum", b)) for b in range(8)]
            self.sem_ld = [None] * 12
            self.sem_w = [None] * 8
            self.sem_st = [None] * 6
            self.sem_c = None
            self.sem_cp = None
            self.setup_consts()
            self.const_mark = self.arena.off

            stages = [
                ("A", self.stage_A), ("B", self.stage_B), ("C", self.stage_C), ("D", self.stage_D),
                ("E", self.stage_E), ("F", self.stage_F), ("G", lambda: self.stage_G(out)),
            ]
            for name, fn in stages:
                self.arena.off = self.const_mark
                fn()
                self.P.barrier()
                if self.stop_after == name:
                    break
            self.P.barrier()
            self.P.emit()
        return nc

    def setup_consts(self):
        A = self.arena
        cb = A.alloc(7 * 128, BF16).rearrange("p (a b) -> p a b", a=7)
        cf = A.alloc(7 * 128, F32).rearrange("p (a b) -> p a b", a=7)
        self.cR = self.newR("consts", track=False)
        src = self.I["cst"].rearrange("p (a b) -> p a b", a=7)
        self.dma(POOL, cb, src, self.sem_cp, [], [self.cR])
        self.dma(SP, cf, src, self.sem_c, [], [self.cR])
        self.cb, self.cf = cb, cf
        self.sel_sb = A.alloc(2, F32)
        self.eps_t = A.alloc(1, F32)
        self.P.add(DVE, lambda e: e.memset(self.eps_t, EPS), [], [self.cR])
        self.dma(SP, self.sel_sb, self.I["sel"], self.sem_c, [], [self.cR])

    def gain_bcast(self, gap, q=SP):
        t = self.arena.alloc(D, F32)
        r = self.newR("gain", track=False)
        self.dma(q, t, gap.unsqueeze(0).broadcast_to([128, D]), self.sem_c, [], [r])
        return t, r

    def rmsnorm_tile(self, src, srcR, gB, gR, out, outR, junk, junkR, stat, statR):
        self.P.add(DVE, lambda e: e.memset(stat[:, 0:1], 0.0), [], [statR])
        self.act(junk, src, AF.Square, [srcR, statR], [junkR, statR], accum=stat[:, 0:1])
        self.act(stat[:, 1:2], stat[:, 0:1], AF.Sqrt, [statR, self.cR], [statR], bias=self.eps_t, scale=1.0 / D)
        self.P.add(DVE, lambda e: e.reciprocal(out=stat[:, 1:2], in_=stat[:, 1:2]), [statR], [statR])
        self.stt(out, src, stat[:, 1:2], gB, ALU.mult, ALU.mult, [srcR, statR, gR], [outR])

    def proj_phase(self, tag, ntiles, src_fn, gain_ap, W_ap, ncols, fm_outs, tm_outs):
        A, P = self.arena, self.P
        cR = self.cR
        W_sb = A.alloc(8 * ncols, BF16).rearrange("p (k n) -> p k n", k=8)
        WR = self.newR(tag + "W", track=False)
        Wv = W_ap.rearrange("(k p) n -> p k n", p=128)
        for k in range(8):
            self.dma(POOL, W_sb[:, k, :], Wv[:, k, :], self.sem_w[k % 4], [], [WR])
        gB, gR = self.gain_bcast(gain_ap)
        GT = min(4, ntiles)
        NG = ntiles // GT
        GW = GT * 128
        xs = [A.alloc(D, F32) for _ in range(3)]
        xsR = [self.newR(tag + "xs%d" % i) for i in range(3)]
        xs2 = A.alloc(D, F32)
        xs2R = self.newR(tag + "xs2")
        xn = [A.alloc(D, BF16) for _ in range(2)]
        xnR = [self.newR(tag + "xn%d" % i) for i in range(2)]
        junk = A.alloc(D, BF16)
        junkR = self.newR(tag + "junk")
        stat = [A.alloc(2, F32) for _ in range(2)]
        statR = [self.newR(tag + "stat%d" % i) for i in range(2)]
        xnT = [A.alloc(8 * GW, BF16).rearrange("p (k t) -> p k t", k=8) for _ in range(2)]
        xnTR = [self.newR(tag + "xnT%d" % i) for i in range(2)]
        stg = [A.alloc(512, BF16) for _ in range(3)]
        stgR = [self.newR(tag + "stg%d" % i) for i in range(3)]
        need_rope = any(o.get("rope") is not None for o in fm_outs)
        if need_rope:
            qf = [A.alloc(GW, F32) for _ in range(2)]
            qfR = [self.newR(tag + "qf%d" % i) for i in range(2)]
            t1 = A.alloc(GW, F32)
            t1R = self.newR(tag + "t1")
            rt = [A.alloc(2 * GW, F32).rearrange("p (a t) -> p a t", a=2) for _ in range(2)]
            rtR = [self.newR(tag + "rt%d" % i) for i in range(2)]
        ps = self.ps
        n_ld = 0
        n_acc = 0
        n_stg = 0
        n_tr = 0
        n_qf = 0
        for g in range(NG):
            xT = xnT[g % 2]
            xTR = xnTR[g % 2]
            for tl in range(GT):
                ti = g * GT + tl
                srcs = src_fn(ti)
                s0 = xs[n_ld % 3]
                s0R = xsR[n_ld % 3]
                ld = self.sem_ld[n_ld % 3]
                n_ld += 1
                self.dma(SP, s0, srcs[0][0], ld, [], [s0R])
                if len(srcs) == 2:
                    self.dma(SP, xs2, srcs[1][0], self.sem_ld[3], [], [xs2R])
                    self.ts(s0, s0, self.sel_sb[:, 0:1], None, ALU.mult, None, [s0R, cR], [s0R])
                    self.stt(s0, xs2, self.sel_sb[:, 1:2], s0, ALU.mult, ALU.add, [xs2R, s0R, cR], [s0R])
                    if srcs[0][1] is not None:
                        self.dma(SP, srcs[0][1], s0, self.sem_st[0], [s0R], [self.R("hown", ti)])
                xo = xn[ti % 2]
                xoR = xnR[ti % 2]
                self.rmsnorm_tile(s0, s0R, gB, gR, xo, xoR, junk, junkR, stat[ti % 2], statR[ti % 2])
                bank = 6 + (n_tr % 2)
                n_tr += 1
                pb = ps[:, bank, :].bitcast(BF16).rearrange("p (k t) -> p k t", k=8)
                for k in range(8):
                    self.tr(pb[:, k, :], xo[:, k * 128:(k + 1) * 128], self.cb[:, C_ID, :], [xoR, cR], [self.psR[bank]])
                self.P.add(ACT, lambda e, o=xT[:, :, tl * 128:(tl + 1) * 128], i=pb: e.copy(out=o, in_=i),
                           [self.psR[bank]], [xTR])
            for o in fm_outs:
                if o.get("rope") is not None:
                    r_t = rt[g % 2]
                    r_R = rtR[g % 2]
                    self.dma(SP, r_t, o["rope"][:, :, g * GW:(g + 1) * GW].rearrange("a p t -> p a t"),
                             self.sem_ld[4 + g % 2], [], [r_R])
                for fo in range(8):
                    bank = n_acc % 3
                    n_acc += 1
                    pa = ps[:, bank, 0:GW]
                    for k in range(8):
                        self.mm(pa, W_sb[:, k, o["col0"] + fo * 128:o["col0"] + (fo + 1) * 128], xT[:, k, :],
                                k == 0, k == 7, [WR, xTR], [self.psR[bank]])
                    sg = stg[n_stg % 3][:, 0:GW]
                    sgR = stgR[n_stg % 3]
                    n_stg += 1
                    if o.get("rope") is None:
                        self.act(sg, pa, AF.Copy, [self.psR[bank]], [sgR], scale=float(o.get("scale", 1.0)))
                    else:
                        qq = qf[n_qf % 2]
                        qqR = qfR[n_qf % 2]
                        n_qf += 1
                        self.act(qq, pa, AF.Copy, [self.psR[bank]], [qqR], scale=float(o.get("scale", 1.0)))
                        rb = 3 + (n_qf % 2)
                        pr = ps[:, rb, 0:GW]
                        self.mm(pr, self.cf[:, C_RT, :], qq, True, True, [cR, qqR], [self.psR[rb]])
                        self.tt(t1, qq, r_t[:, 0, :], ALU.mult, [qqR, r_R], [t1R])
                        self.tt(qq, pr, r_t[:, 1, :], ALU.mult, [self.psR[rb], r_R], [qqR])
                        self.tt(sg, t1, qq, ALU.add, [t1R, qqR], [sgR])
                    self.dma(POOL, o["dst"][fo * 128:(fo + 1) * 128, g * GW:(g + 1) * GW], sg,
                             self.sem_st[1], [sgR], [self.R(o["name"], "g", g)])
            for o in tm_outs:
                for tl in range(GT):
                    ti = g * GT + tl
                    for hf in range(2):
                        bank = n_acc % 3
                        n_acc += 1
                        pa = ps[:, bank, :]
                        for k in range(8):
                            self.mm(pa, xT[:, k, tl * 128:(tl + 1) * 128],
                                    W_sb[:, k, o["col0"] + hf * 512:o["col0"] + (hf + 1) * 512],
                                    k == 0, k == 7, [WR, xTR], [self.psR[bank]])
                        sg = stg[n_stg % 3]
                        sgR = stgR[n_stg % 3]
                        n_stg += 1
                        self.cp(sg, pa, [self.psR[bank]], [sgR])
                        self.dma(POOL, o["dst"][ti * 128:(ti + 1) * 128, hf * 512:(hf + 1) * 512], sg,
                                 self.sem_st[2], [sgR], [self.R(o["name"], "t", ti)])

    def stage_A(self):
        I, X = self.I, self.X
        self.proj_phase(
            "A", self.NTS, lambda ti: [(I["x"][ti * 128:(ti + 1) * 128, :], None)], I["sb_norm_g"],
            I["sb_w_qkv"], 3 * D,
            fm_outs=[dict(name="q0T", col0=0, scale=0.125, dst=X["q0T"]),
                     dict(name="k0T", col0=D, dst=X["k0T"])],
            tm_outs=[dict(name="v0", col0=2 * D, dst=X["v0"])])

    def stage_B(self):
        A, P, ps, cR = self.arena, self.P, self.ps, self.cR
        S, NTS = self.S, self.NTS
        X = self.X
        cb = self.cb
        kT = A.alloc(2 * S, BF16).rearrange("p (c t) -> p c t", c=2)
        qT = A.alloc(2 * NTS * 256, BF16).rearrange("p (c t w) -> p c t w", c=2, t=NTS)
        vv = A.alloc(NTS * 256, BF16).rearrange("p (b d) -> p b d", b=NTS)
        kR, qR, vR = self.newR("B_k"), self.newR("B_q"), self.newR("B_v")
        self.P.add(DVE, lambda e: e.memset(qT, 0.0), [], [qR])
        NB = 4
        e_sb = [A.alloc(512, F32) for _ in range(NB)]
        eR = [self.newR("B_e%d" % i) for i in range(NB)]
        sp_sb = [A.alloc(512, BF16) for _ in range(NB)]
        spR = [self.newR("B_sp%d" % i) for i in range(NB)]
        x_sb = [A.alloc(512, F32) for _ in range(2)]
        xR = [self.newR("B_x%d" % i) for i in range(2)]
        w_sb = [A.alloc(512, BF16) for _ in range(2)]
        wR = [self.newR("B_w%d" % i) for i in range(2)]
        o_sb = [A.alloc(512, BF16) for _ in range(2)]
        oR = [self.newR("B_o%d" % i) for i in range(2)]
        ss_sb = [A.alloc(512, BF16) for _ in range(2)]
        ssR = [self.newR("B_ss%d" % i) for i in range(2)]
        maskS4 = cb[:, C_MS, :].unsqueeze(1).broadcast_to([128, 4, 128])
        all_src = [self.R("q0T", "g", g) for g in range(max(1, NTS // 4))] + \
                  [self.R("k0T", "g", g) for g in range(max(1, NTS // 4))] + \
                  [self.R("v0", "t", t) for t in range(NTS)]
        gbase = 0
        for hg in range(4):
            for c in range(2):
                self.dma(SP, kT[:, c, :], X["k0T"][(hg * 2 + c) * 128:(hg * 2 + c + 1) * 128, :], None,
                         all_src, [kR])
                r0 = (hg * 2 + c) * 128
                for t0 in range(0, NTS, 8):
                    t1 = min(NTS, t0 + 8)
                    self.dma(SP, qT[0:64, c, t0:t1, 0:128],
                             X["q0T"][r0:r0 + 64, t0 * 128:t1 * 128].rearrange("p (t w) -> p t w", w=128),
                             None, all_src, [qR])
                    self.dma(SP, qT[64:128, c, t0:t1, 128:256],
                             X["q0T"][r0 + 64:r0 + 128, t0 * 128:t1 * 128].rearrange("p (t w) -> p t w", w=128),
                             None, all_src, [qR])
            for b0 in range(0, NTS, 4):
                b1 = min(NTS, b0 + 4)
                self.dma(SP, vv[:, b0:b1, :],
                         X["v0"][b0 * 128:b1 * 128, hg * 256:(hg + 1) * 256].rearrange("(b p) d -> p b d", p=128),
                         None, all_src, [vR])
            steps = [(qi, kb, i) for qi in range(NTS) for i, kb in enumerate(range(qi, -1, -1))]
            N = len(steps)

            def S1(k):
                qi, kb, i = steps[k]
                g = gbase + k
                zb = g % 2
                z = ps[:, zb, :]
                for cc in range(2):
                    self.mm(z[:, cc * 256:(cc + 1) * 256], kT[:, cc, kb * 128:(kb + 1) * 128],
                            qT[:, cc, qi, :], True, True, [kR, qR], [self.psR[zb]])

            def S2(k):
                qi, kb, i = steps[k]
                g = gbase + k
                zb, b = g % 2, g % NB
                self.act(e_sb[b], ps[:, zb, :], AF.Exp, [self.psR[zb]], [eR[b]])
                self.act(sp_sb[b], e_sb[b], AF.Ln, [eR[b]], [spR[b]], bias=1.0)
                if i == 0:
                    self.tt(sp_sb[b].rearrange("p (h t) -> p h t", h=4),
                            sp_sb[b].rearrange("p (h t) -> p h t", h=4), maskS4, ALU.mult,
                            [spR[b], cR], [spR[b]])

            def S3(k):
                qi, kb, i = steps[k]
                g = gbase + k
                b = g % NB
                last = (kb == 0)
                pbank = 2 + (g % 2)
                Pp = ps[:, pbank, :]
                if i == 0:
                    self.mm(Pp, cb[:, C_TRI, :], sp_sb[b], True, True, [cR, spR[b]], [self.psR[pbank]])
                else:
                    if i == 1:
                        car, carR = sp_sb[(g - 1) % NB], spR[(g - 1) % NB]
                    else:
                        car, carR = ss_sb[i % 2], ssR[i % 2]
                    self.mm(Pp, cb[:, C_TRI, :], sp_sb[b], True, False, [cR, spR[b]], [self.psR[pbank]])
                    self.mm(Pp, cb[:, C_ONE, :], car, False, True, [cR, carR], [self.psR[pbank]])
                    if not last:
                        self.tt(ss_sb[(i + 1) % 2], car, sp_sb[b], ALU.add, [carR, spR[b]], [ssR[(i + 1) % 2]])

            def S4(k):
                g = gbase + k
                pbank = 2 + (g % 2)
                self.act(x_sb[g % 2], ps[:, pbank, :], AF.Exp, [self.psR[pbank]], [xR[g % 2]], scale=-1.0)

            def S5(k):
                qi, kb, i = steps[k]
                g = gbase + k
                b, b2 = g % NB, g % 2
                self.tt(w_sb[b2], e_sb[b], x_sb[b2], ALU.mult, [eR[b], xR[b2]], [wR[b2]])
                if i == 0:
                    self.tt(w_sb[b2].rearrange("p (h t) -> p h t", h=4),
                            w_sb[b2].rearrange("p (h t) -> p h t", h=4), maskS4, ALU.mult,
                            [wR[b2], cR], [wR[b2]])

            def S6(k):
                qi, kb, i = steps[k]
                g = gbase + k
                b2 = g % 2
                first, last = (i == 0), (kb == 0)
                ob0 = 4 + 2 * (qi % 2)
                for pr in range(2):
                    self.mm(ps[:, ob0 + pr, 0:256], vv[:, kb, pr * 128:(pr + 1) * 128],
                            w_sb[b2][:, pr * 256:(pr + 1) * 256], first, last, [vR, wR[b2]],
                            [self.psR[ob0 + pr]])
                if last:
                    ob = o_sb[qi % 2]
                    self.cp(ob.rearrange("p (r c) -> p r c", r=2), ps[:, ob0:ob0 + 2, 0:256],
                            [self.psR[ob0], self.psR[ob0 + 1]], [oR[qi % 2]])
                    for pr in range(2):
                        for hf in range(2):
                            r0_ = hg * 256 + pr * 128 + hf * 64
                            self.dma(POOL, X["a0T"][r0_:r0_ + 64, qi * 128:(qi + 1) * 128],
                                     ob[hf * 64:(hf + 1) * 64, pr * 256 + hf * 128:pr * 256 + (hf + 1) * 128], None,
                                     [oR[qi % 2]], [self.R("a0T", qi // 4)])

            stages = (S1, S2, S3, S4, S5, S6)
            for it in range(-5, N):
                for si, fn in enumerate(stages):
                    k = it + 5 - si
                    if 0 <= k < N:
                        fn(k)
            gbase += N

    def tail_phase(self, tag, ntiles, aT, aT_res, Wo_ap, resid_fn, resid_res, gain_ap, experts, router_ap,
                   final_gain_ap, dst, dst_name):
        A, P, ps, cR = self.arena, self.P, self.ps, self.cR
        cb, cf = self.cb, self.cf
        GT = min(4, ntiles)
        NG = ntiles // GT
        GW = GT * 128
        Wo = A.alloc(8 * D, BF16).rearrange("p (k n) -> p k n", k=8)
        WoR = self.newR(tag + "Wo", track=False)
        Wov = Wo_ap.rearrange("(k p) n -> p k n", p=128)
        for k in range(8):
            self.dma(POOL, Wo[:, k, :], Wov[:, k, :], self.sem_w[4 + k % 2], [], [WoR])
        gB, gR = self.gain_bcast(gain_ap)
        if final_gain_ap is not None:
            gF, gFR = self.gain_bcast(final_gain_ap)
        if router_ap is not None:
            wr = A.alloc(8 * N_EXP, F32).rearrange("p (k n) -> p k n", k=8)
            wrR = self.newR(tag + "wr", track=False)
            self.dma(SP, wr, router_ap.rearrange("(k p) n -> p k n", p=128), self.sem_c, [], [wrR])
            hnT32 = A.alloc(8 * 128, F32).rearrange("p (k t) -> p k t", k=8)
            hnT32R = self.newR(tag + "hnT32")
            comb = A.alloc(GT * N_EXP, F32).rearrange("p (t n) -> p t n", t=GT)
            combR = self.newR(tag + "comb")
            rw = A.alloc(64, F32)
            rwR = self.newR(tag + "rw")
        aT_sb = A.alloc(8 * GW, BF16).rearrange("p (k t) -> p k t", k=8)
        aTR = self.newR(tag + "aT")
        acc = A.alloc(GT * D, F32).rearrange("p (t n) -> p t n", t=GT)
        accR = [self.newR(tag + "acc%d" % i) for i in range(GT)]
        rs = [A.alloc(D, F32) for _ in range(2)]
        rsR = [self.newR(tag + "rs%d" % i) for i in range(2)]
        hn = A.alloc(D, F32 if router_ap is not None else BF16)
        hnR = self.newR(tag + "hn")
        junk = A.alloc(D, BF16)
        junkR = self.newR(tag + "junk")
        stat = A.alloc(2, F32)
        statR = self.newR(tag + "stat")
        hnT = A.alloc(8 * GW, BF16).rearrange("p (k t) -> p k t", k=8)
        hnTR = self.newR(tag + "hnT")
        NFmax = max(F for (_, _, F) in experts) // 128
        actT = A.alloc(NFmax * GW, BF16).rearrange("p (f t) -> p f t", f=NFmax)
        actR = self.newR(tag + "actT")
        sg = [A.alloc(GW, F32) for _ in range(2)]
        sgR = [self.newR(tag + "sg%d" % i) for i in range(2)]
        NWS = 3
        wgu = [A.alloc(8 * 2 * 512, BF16).rearrange("p (k a n) -> p k a n", k=8, a=2) for _ in range(NWS)]
        wguR = [self.newR(tag + "wgu%d" % i) for i in range(NWS)]
        NDS = 2
        NBDmax = max((7 if (F // 128) % 7 == 0 else 11) for (_, _, F) in experts)
        wd = [A.alloc(NBDmax * D, BF16).rearrange("p (c n) -> p c n", c=NBDmax) for _ in range(NDS)]
        wdR = [self.newR(tag + "wd%d" % i) for i in range(NDS)]
        ot, otR = rs, rsR
        n_acc = 0
        n_gu = 0
        n_wgu = 0
        n_wd = 0
        n_sg = 0
        for g in range(NG):
            self.dma(SP, aT_sb, aT[:, g * GW:(g + 1) * GW].rearrange("(k p) t -> p k t", p=128), self.sem_ld[6],
                     aT_res(g), [aTR])
            for tl in range(GT):
                ti = g * GT + tl
                r_t, r_R = rs[ti % 2], rsR[ti % 2]
                self.dma(SP, r_t, resid_fn(ti), self.sem_ld[7 + ti % 2], resid_res(ti), [r_R])
                for hf in range(2):
                    bank = n_acc % 2
                    n_acc += 1
                    pa = ps[:, bank, :]
                    for k in range(8):
                        self.mm(pa, aT_sb[:, k, tl * 128:(tl + 1) * 128], Wo[:, k, hf * 512:(hf + 1) * 512],
                                k == 0, k == 7, [aTR, WoR], [self.psR[bank]])
                    self.tt(acc[:, tl, hf * 512:(hf + 1) * 512], pa, r_t[:, hf * 512:(hf + 1) * 512], ALU.add,
                            [self.psR[bank], r_R], [accR[tl]])
                self.rmsnorm_tile(acc[:, tl, :], accR[tl], gB, gR, hn, hnR, junk, junkR, stat, statR)
                if router_ap is None:
                    bank = 6 + (ti % 2)
                    pb = ps[:, bank, :].bitcast(BF16).rearrange("p (k t) -> p k t", k=8)
                    for k in range(8):
                        self.tr(pb[:, k, :], hn[:, k * 128:(k + 1) * 128], cb[:, C_ID, :], [hnR, cR], [self.psR[bank]])
                    self.P.add(ACT, lambda e, o=hnT[:, :, tl * 128:(tl + 1) * 128], i=pb: e.copy(out=o, in_=i),
                               [self.psR[bank]], [hnTR])
                else:
                    p32 = ps[:, 6:8, :].rearrange("p b (k t) -> p (b k) t", k=4)
                    for k in range(8):
                        self.tr(p32[:, k, :], hn[:, k * 128:(k + 1) * 128], cf[:, C_ID, :], [hnR, cR],
                                [self.psR[6], self.psR[7]])
                    self.P.add(ACT, lambda e, o=hnT32, i=p32: e.copy(out=o, in_=i), [self.psR[6], self.psR[7]], [hnT32R])
                    self.cp(hnT[:, :, tl * 128:(tl + 1) * 128], hnT32, [hnT32R], [hnTR])
                    bank = n_acc % 2
                    n_acc += 1
                    pl = ps[:, bank, 0:N_EXP]
                    for k in range(8):
                        self.mm(pl, hnT32[:, k, :], wr[:, k, :], k == 0, k == 7, [hnT32R, wrR], [self.psR[bank]])
                    L, E1, L2, E2, SC, C1 = (rw[:, i * 8:(i + 1) * 8] for i in range(6))
                    rr = [rwR]
                    self.cp(L, pl, [self.psR[bank]], rr)
                    P.add(DVE, lambda e, o=SC[:, 0:1], i=L: e.tensor_reduce(out=o, in_=i, axis=AX.X, op=ALU.max), rr, rr)
                    self.ts(E1, L, SC[:, 0:1], None, ALU.is_equal, None, rr, rr)
                    self.stt(L2, E1, -1e30, L, ALU.mult, ALU.add, rr, rr)
                    P.add(DVE, lambda e, o=SC[:, 1:2], i=L2: e.tensor_reduce(out=o, in_=i, axis=AX.X, op=ALU.max), rr, rr)
                    self.ts(E2, L2, SC[:, 1:2], None, ALU.is_equal, None, rr, rr)
                    self.tt(SC[:, 2:3], SC[:, 1:2], SC[:, 0:1], ALU.subtract, rr, rr)
                    self.act(SC[:, 3:4], SC[:, 2:3], AF.Exp, rr, rr)
                    self.ts(SC[:, 4:5], SC[:, 3:4], 1.0, None, ALU.add, None, rr, rr)
                    P.add(DVE, lambda e, o=SC[:, 5:6], i=SC[:, 4:5]: e.reciprocal(out=o, in_=i), rr, rr)
                    self.tt(SC[:, 6:7], SC[:, 3:4], SC[:, 5:6], ALU.mult, rr, rr)
                    self.ts(C1, E1, SC[:, 5:6], None, ALU.mult, None, rr, rr)
                    self.stt(comb[:, tl, :], E2, SC[:, 6:7], C1, ALU.mult, ALU.add, rr, [combR])
            for ei, (Wgu_ap, Wd_ap, F) in enumerate(experts):
                NF = F // 128
                BF = 4 if NF % 4 == 0 else 2
                Wg_v = Wgu_ap.rearrange("(k p) n -> p k n", p=128)
                for blk in range(NF // BF):
                    s = n_wgu % NWS
                    n_wgu += 1
                    wsl, wslR = wgu[s], wguR[s]
                    bw = BF * 128
                    self.dma(POOL, wsl[:, :, 0, 0:bw], Wg_v[:, :, blk * bw:(blk + 1) * bw], self.sem_w[s], [], [wslR])
                    self.dma(POOL, wsl[:, :, 1, 0:bw], Wg_v[:, :, F + blk * bw:F + (blk + 1) * bw], self.sem_w[s], [],
                             [wslR])
                    for c in range(BF):
                        fc = blk * BF + c
                        gb = 2 + (n_gu % 2)
                        ub = 4 + (n_gu % 2)
                        n_gu += 1
                        pg = ps[:, gb, 0:GW]
                        pu = ps[:, ub, 0:GW]
                        for k in range(8):
                            self.mm(pg, wsl[:, k, 0, c * 128:(c + 1) * 128], hnT[:, k, :], k == 0, k == 7,
                                    [wslR, hnTR], [self.psR[gb]])
                        for k in range(8):
                            self.mm(pu, wsl[:, k, 1, c * 128:(c + 1) * 128], hnT[:, k, :], k == 0, k == 7,
                                    [wslR, hnTR], [self.psR[ub]])
                        s_t, s_R = sg[n_sg % 2], sgR[n_sg % 2]
                        n_sg += 1
                        self.act(s_t, pg, AF.Silu, [self.psR[gb]], [s_R])
                        self.tt(actT[:, fc, :], s_t, pu, ALU.mult, [s_R, self.psR[ub]], [actR])
                NBD = 7 if NF % 7 == 0 else 11
                Wd_v = Wd_ap.rearrange("(c p) n -> p c n", p=128)
                for blk in range(NF // NBD):
                    s = n_wd % NDS
                    n_wd += 1
                    dsl, dslR = wd[s], wdR[s]
                    self.dma(POOL, dsl[:, 0:NBD, :], Wd_v[:, blk * NBD:(blk + 1) * NBD, :], self.sem_w[4 + s], [], [dslR])
                    for tl in range(GT):
                        for hf in range(2):
                            bank = n_acc % 2
                            n_acc += 1
                            pa = ps[:, bank, :]
                            for c in range(NBD):
                                self.mm(pa, actT[:, blk * NBD + c, tl * 128:(tl + 1) * 128],
                                        dsl[:, c, hf * 512:(hf + 1) * 512], c == 0, c == NBD - 1,
                                        [actR, dslR], [self.psR[bank]])
                            av = acc[:, tl, hf * 512:(hf + 1) * 512]
                            if router_ap is None:
                                self.tt(av, pa, av, ALU.add, [self.psR[bank], accR[tl]], [accR[tl]])
                            else:
                                self.stt(av, pa, comb[:, tl, ei:ei + 1], av, ALU.mult, ALU.add,
                                         [self.psR[bank], accR[tl], combR], [accR[tl]])
            for tl in range(GT):
                ti = g * GT + tl
                if final_gain_ap is None:
                    self.dma(SP, dst[ti * 128:(ti + 1) * 128, :], acc[:, tl, :], self.sem_st[4], [accR[tl]],
                             [self.R(dst_name, ti)])
                else:
                    o_t, o_R = ot[ti % 2], otR[ti % 2]
                    self.rmsnorm_tile(acc[:, tl, :], accR[tl], gF, gFR, o_t, o_R, junk, junkR, stat, statR)
                    self.dma(SP, dst[ti * 128:(ti + 1) * 128, :], o_t, self.sem_st[4], [o_R], [self.R(dst_name, ti)])

    def stage_C(self):
        I, X = self.I, self.X
        self.tail_phase(
            "C", self.NTS, X["a0T"], lambda g: [self.R("a0T", g)], I["sb_w_o"],
            lambda ti: I["x"][ti * 128:(ti + 1) * 128, :], lambda ti: [],
            I["ffn_norm_g"][0], [(I["dense_w_gate_up"], I["dense_w_down"], F_DENSE)], None, None,
            X["h1"], "h1")

    def stage_D(self):
        I, X = self.I, self.X
        self.proj_phase(
            "D", self.NTS, lambda ti: [(X["h1"][ti * 128:(ti + 1) * 128, :], None)], I["kv_norm_g"],
            I["diff_w_kv"], 2 * D,
            fm_outs=[dict(name="k1T", col0=0, dst=X["k1T"], rope=I["ropek"])],
            tm_outs=[dict(name="v1", col0=D, dst=X["v1"])])

    def stage_E(self):
        I, X = self.I, self.X
        gm = tile_map(self.S)

        def src(m):
            a, b = gm[0][m], gm[1][m]
            return [(X["h1"][a * 128:(a + 1) * 128, :], X["hown"][m * 128:(m + 1) * 128, :]),
                    (X["h1"][b * 128:(b + 1) * 128, :], None)]

        self.proj_phase(
            "E", self.NT, src, I["diff_norm_g"], I["diff_w_q"], D,
            fm_outs=[dict(name="q1T", col0=0, dst=X["q1T"], rope=I["ropeq"], scale=0.125)], tm_outs=[])

    def stage_F(self):
        A, P, ps, cR = self.arena, self.P, self.ps, self.cR
        S, NTS, NT, TOK = self.S, self.NTS, self.NT, self.TOK
        I, X = self.I, self.X
        cb, cf = self.cb, self.cf
        lv = A.alloc(4 * 64, F32).rearrange("p (a d) -> p a d", a=4)
        lw = A.alloc(8, F32)
        lR = self.newR("F_lam")
        self.dma(SP, lv, I["lam"].unsqueeze(0).broadcast_to([128, 4, 64]), self.sem_c, [], [lR])
        pr = A.alloc(128, F32).rearrange("p (a d) -> p a d", a=2)
        self.tt(pr[:, 0, :], lv[:, 0, :], lv[:, 1, :], ALU.mult, [lR], [lR])
        self.tt(pr[:, 1, :], lv[:, 2, :], lv[:, 3, :], ALU.mult, [lR], [lR])
        P.add(DVE, lambda e: e.tensor_reduce(out=lw[:, 0:2], in_=pr, axis=AX.X, op=ALU.add), [lR], [lR])
        self.act(lw[:, 2:4], lw[:, 0:2], AF.Exp, [lR], [lR])
        lam_init = 0.8 - 0.6 * math.exp(-0.3 * 1)
        self.tt(lw[:, 4:5], lw[:, 3:4], lw[:, 2:3], ALU.subtract, [lR], [lR])
        self.ts(lw[:, 5:6], lw[:, 4:5], -float(lam_init), None, ALU.add, None, [lR], [lR])
        neg_lam = lw[:, 5:6]
        gs = A.alloc(2, F32)
        self.dma(SP, gs[:, 0:1], I["diff_subln_g"], self.sem_c, [], [lR])
        self.ts(gs[:, 1:2], gs[:, 0:1], float(1.0 - lam_init), None, ALU.mult, None, [lR], [lR])
        gsub = gs[:, 1:2]
        kT = A.alloc(2 * S, BF16).rearrange("p (c t) -> p c t", c=2)
        qT = A.alloc(2 * NT * 256, BF16).rearrange("p (c t w) -> p c t w", c=2, t=NT)
        vv = A.alloc(NTS * 256, BF16).rearrange("p (b d) -> p b d", b=NTS)
        kR, qR, vR = self.newR("F_k"), self.newR("F_q"), self.newR("F_v")
        self.P.add(DVE, lambda e: e.memset(qT, 0.0), [], [qR])
        mk = A.alloc(NT * 2 * 128, BF16).rearrange("p (m a t) -> p m a t", m=NT, a=2)
        mkR = self.newR("F_mask", track=False)
        for m0 in range(0, NT, 2):
            m1 = min(NT, m0 + 2)
            self.dma(POOL, mk[:, m0:m1], I["mask1"][m0:m1].rearrange("m a s t -> s m a t"), None, [], [mkR])
        e_sb = [A.alloc(512, BF16) for _ in range(3)]
        eR = [self.newR("F_e%d" % i) for i in range(3)]
        f1 = A.alloc(512, F32)
        f2 = A.alloc(512, F32)
        f3 = A.alloc(256, F32)
        f4 = A.alloc(256, F32)
        fR = self.newR("F_fin")
        on = [A.alloc(256, BF16) for _ in range(2)]
        onR = [self.newR("F_on%d" % i) for i in range(2)]
        src_all = [self.R("k1T", "g", g) for g in range(max(1, NTS // 4))] + \
                  [self.R("v1", "t", t) for t in range(NTS)] + \
                  [self.R("q1T", "g", g) for g in range(max(1, NT // 4))]
        gbase = 0
        for hp in range(4):
            for c in range(2):
                h = hp * 2 + c
                self.dma(SP, kT[:, c, :], X["k1T"][h * 128:(h + 1) * 128, :], self.sem_ld[0], src_all, [kR])
                for t0 in range(0, NT, 8):
                    t1 = min(NT, t0 + 8)
                    self.dma(SP, qT[0:64, c, t0:t1, 0:128],
                             X["q1T"][h * 128:h * 128 + 64, t0 * 128:t1 * 128].rearrange("p (t w) -> p t w", w=128),
                             None, src_all, [qR])
                    self.dma(SP, qT[64:128, c, t0:t1, 128:256],
                             X["q1T"][h * 128 + 64:(h + 1) * 128, t0 * 128:t1 * 128].rearrange("p (t w) -> p t w", w=128),
                             None, src_all, [qR])
            for b0 in range(0, NTS, 4):
                b1 = min(NTS, b0 + 4)
                self.dma(SP, vv[:, b0:b1, :],
                         X["v1"][b0 * 128:b1 * 128, hp * 256:(hp + 1) * 256].rearrange("(b p) d -> p b d", p=128),
                         None, src_all, [vR])
            steps = []
            for m in range(NT):
                k4, r = divmod(m, 2)
                gmax = 4 * k4 + (1 if r == 0 else 3)
                steps += [(m, kb, gmax) for kb in range(gmax + 1)]
            N = len(steps)

            def S1(k):
                m, kb, gmax = steps[k]
                zb = (gbase + k) % 2
                z = ps[:, zb, :]
                for hh in range(2):
                    self.mm(z[:, hh * 256:(hh + 1) * 256], kT[:, hh, kb * 128:(kb + 1) * 128],
                            qT[:, hh, m, :], True, True, [kR, qR], [self.psR[zb]])

            def S2(k):
                m, kb, gmax = steps[k]
                g = gbase + k
                zb, eb = g % 2, g % 3
                self.act(e_sb[eb], ps[:, zb, :], AF.Exp, [self.psR[zb]], [eR[eb]])
                if kb >= gmax - 1:
                    mm_ = mk[:, m, kb - (gmax - 1), :].unsqueeze(1).broadcast_to([128, 4, 128])
                    ev = e_sb[eb].rearrange("p (h t) -> p h t", h=4)
                    self.tt(ev, ev, mm_, ALU.mult, [eR[eb], mkR], [eR[eb]])

            def S3(k):
                m, kb, gmax = steps[k]
                g = gbase + k
                eb = g % 3
                ob0 = 2 + 2 * (m % 2)
                sbank = 6 + (m % 2)
                Oa = ps[:, ob0:ob0 + 2, 0:256]
                Sa = ps[:, sbank, :]
                for hh in range(2):
                    self.mm(ps[:, ob0 + hh, 0:256], vv[:, kb, hh * 128:(hh + 1) * 128],
                            e_sb[eb][:, hh * 256:(hh + 1) * 256], kb == 0, kb == gmax, [vR, eR[eb]],
                            [self.psR[ob0 + hh]])
                self.mm(Sa, cb[:, C_ONE, :], e_sb[eb], kb == 0, kb == gmax, [cR, eR[eb]], [self.psR[sbank]])
                if kb != gmax:
                    return
                rr = [fR]
                self.act(f1, Sa, AF.Ln, [self.psR[sbank]], rr)
                self.act(f1, f1, AF.Exp, rr, rr, scale=-1.0)
                self.tt(f2.rearrange("p (h c) -> p h c", h=2), Oa, f1.rearrange("p (h c) -> p h c", h=2), ALU.mult,
                        [self.psR[ob0], self.psR[ob0 + 1]] + rr, rr)
                f2v = f2.rearrange("p (h a t) -> p h a t", h=2, a=2)
                f3v = f3.rearrange("p (h t) -> p h t", h=2)
                self.stt(f3v, f2v[:, :, 1, :], neg_lam, f2v[:, :, 0, :], ALU.mult, ALU.add, rr + [lR], rr)
                self.tt(f4, f3, f3, ALU.mult, rr, rr)
                pm = ps[:, sbank, 0:256]
                self.mm(pm, cf[:, C_ONE, :], f4, True, True, [cR] + rr, [self.psR[sbank]])
                self.act(f4, pm, AF.Ln, [self.psR[sbank], cR], rr, bias=self.eps_t, scale=1.0 / 128.0)
                self.act(f4, f4, AF.Exp, rr, rr, scale=-0.5)
                self.tt(f3, f3, f4, ALU.mult, rr, rr)
                o_t, o_R = on[m % 2], onR[m % 2]
                self.ts(o_t, f3, gsub, None, ALU.mult, None, rr + [lR], [o_R])
                dst = X["a1T"][hp * 256:(hp + 1) * 256, m * 128:(m + 1) * 128].rearrange("(h d) t -> d h t", d=128)
                self.dma(POOL, dst, o_t.rearrange("p (h t) -> p h t", h=2), None, [o_R], [self.R("a1T", m // 4)])

            for it in range(-2, N):
                for si, fn in enumerate((S1, S2, S3)):
                    k = it + 2 - si
                    if 0 <= k < N:
                        fn(k)
            gbase += N

    def stage_G(self, out):
        I, X = self.I, self.X
        experts = [(I["moe_w_gate_up"][e], I["moe_w_down"][e], F_EXP) for e in range(N_EXP)]
        self.tail_phase(
            "G", self.NT, X["a1T"], lambda g: [self.R("a1T", g)], I["diff_w_o"],
            lambda ti: X["hown"][ti * 128:(ti + 1) * 128, :], lambda ti: [self.R("hown", ti)],
            I["ffn_norm_g"][1], experts, I["moe_w_router"], I["final_norm_g"], out, "out")


def make_in_maps(inputs, S):
    gm = tile_map(S)
    NT = S // 256
    cst = make_consts()
    ropek = rope_tables(np.arange(S))
    i = np.arange(128)
    ones = np.ones((128, 128), np.float32)
    zeros = np.zeros((128, 128), np.float32)
    diag = (i[:, None] <= i[None, :]).astype(np.float32)
    f = lambda a: np.ascontiguousarray(np.asarray(a, dtype=np.float32))
    shared = {
        "cst": cst, "ropek": ropek,
        "sb_norm_g": f(inputs["sb_norm_g"][0]), "sb_w_qkv": f(inputs["sb_w_qkv"][0]), "sb_w_o": f(inputs["sb_w_o"][0]),
        "kv_norm_g": f(inputs["kv_norm_g"]), "diff_w_kv": f(inputs["diff_w_kv"]),
        "diff_norm_g": f(inputs["diff_norm_g"][0]), "diff_w_q": f(inputs["diff_w_q"][0]),
        "lam": f(np.stack([inputs["diff_lambda_q1"][0], inputs["diff_lambda_k1"][0],
                           inputs["diff_lambda_q2"][0], inputs["diff_lambda_k2"][0]], 0)),
        "diff_subln_g": f(inputs["diff_subln_g"][0]).reshape(128, 1),
        "diff_w_o": f(inputs["diff_w_o"][0]), "ffn_norm_g": f(inputs["ffn_norm_g"]),
        "dense_w_gate_up": f(inputs["dense_w_gate_up"][0]), "dense_w_down": f(inputs["dense_w_down"][0]),
        "moe_w_router": f(inputs["moe_w_router"][0]), "moe_w_gate_up": f(inputs["moe_w_gate_up"][0]),
        "moe_w_down": f(inputs["moe_w_down"][0]), "final_norm_g": f(inputs["final_norm_g"]),
    }
    x = np.asarray(inputs["x"], dtype=np.float32)
    maps = []
    for c in range(8):
        b, j = divmod(c, 2)
        pos = np.concatenate([gm[j][m] * 128 + np.arange(128) for m in range(NT)])
        mask1 = np.zeros((NT, 2, 128, 128), np.float32)
        for m in range(NT):
            k4, r = divmod(m, 2)
            gmax = 4 * k4 + (1 if r == 0 else 3)
            g = gm[j][m]
            for a, kb in enumerate((gmax - 1, gmax)):
                mask1[m, a] = ones if kb < g else (diag if kb == g else zeros)
        sel = np.zeros((128, 2), np.float32)
        sel[:, j] = 1.0
        d = dict(shared)
        d.update({"x": np.ascontiguousarray(x[b]), "ropeq": rope_tables(pos), "sel": sel, "mask1": mask1})
        maps.append(d)
    return maps


_NC_CACHE = {}


def kernel(**inputs):
    S = int(np.asarray(inputs["x"]).shape[1])
    B = int(np.asarray(inputs["x"]).shape[0])
    assert B == 4
    if S not in _NC_CACHE:
        _NC_CACHE[S] = Builder(S).build()
    nc = _NC_CACHE[S]
    maps = make_in_maps(inputs, S)
    res = run_bass_kernel_spmd(nc, maps, core_ids=list(range(8)))
    gm = tile_map(S)
    NT = S // 256
    out = np.zeros((B, S, D), np.float32)
    for c in range(8):
        b, j = divmod(c, 2)
        o = np.asarray(res.results[c]["out"]).reshape(NT, 128, D)
        for m in range(NT):
            g = gm[j][m]
            out[b, g * 128:(g + 1) * 128] = o[m]
    return out
```

```python
import contextlib
import math
import numpy as np
import concourse.bass as bass
import concourse.mybir as mybir
from concourse.bass_utils import run_bass_kernel_spmd

F32 = mybir.dt.float32
BF16 = mybir.dt.bfloat16
AF = mybir.ActivationFunctionType
ALU = mybir.AluOpType
AX = mybir.AxisListType

D = 1024
EPS = 1e-5
F_DENSE = 2816
F_EXP = 3584
N_EXP = 8
ROPE_THETA = 500000.0
ROPE_DIM = 16
PE, ACT, DVE, POOL, SP = "pe", "act", "dve", "pool", "sp"
ENGS = (PE, ACT, DVE, POOL, SP)


class Res:
    __slots__ = ("name", "lw", "rd", "track")

    def __init__(self, name, track=True):
        self.name = name
        self.lw = None
        self.rd = []
        self.track = track


class DSem:
    __slots__ = ("h", "count", "last")

    def __init__(self, h):
        self.h = h
        self.count = 0
        self.last = None


class Op:
    __slots__ = ("eng", "fn", "waits", "inc", "cnt", "dsem", "dval")

    def __init__(self, eng, fn, dsem):
        self.eng = eng
        self.fn = fn
        self.waits = []
        self.inc = False
        self.cnt = 0
        self.dsem = dsem
        self.dval = 0


class Prog:
    def __init__(self, nc, es):
        self.nc = nc
        self.es = es
        self.ops = {e: [] for e in ENGS}
        self.esem = {e: es.enter_context(nc.semaphore("sem_" + e)) for e in (PE, ACT, DVE, POOL)}
        self.dsems = []
        self.last_real = {e: None for e in ENGS}
        self.qsems = {q: [self.dsem("dq_%s_%d" % (q, i)) for i in range(10)] for q in (SP, POOL)}
        self.qctr = {SP: 0, POOL: 0}

    def dsem(self, name):
        d = DSem(self.es.enter_context(self.nc.semaphore(name)))
        self.dsems.append(d)
        return d

    def add(self, eng, fn, reads=(), writes=(), dsem=None):
        deps = []
        if dsem == "auto":
            pool = self.qsems[eng]
            dsem = pool[self.qctr[eng] % len(pool)]
            self.qctr[eng] += 1
            if dsem.last is not None:
                deps.append(dsem.last)
        op = Op(eng, fn, dsem)
        for r in reads:
            if r.lw is not None:
                deps.append(r.lw)
        for w in writes:
            if w.lw is not None:
                deps.append(w.lw)
            deps.extend(w.rd)
        seen = set()
        for d in deps:
            if d is op or id(d) in seen:
                continue
            seen.add(id(d))
            if d.dsem is None and d.eng == PE and eng == PE:
                continue
            op.waits.append(d)
            if d.dsem is None:
                d.inc = True
        for r in reads:
            if r.track:
                r.rd.append(op)
        for w in writes:
            w.lw = op
            w.rd = []
        if dsem is not None:
            dsem.count += 16
            op.dval = dsem.count
            dsem.last = op
        self.ops[eng].append(op)
        if fn is not None:
            self.last_real[eng] = op
        return op

    def barrier(self):
        lasts = [self.last_real[e] for e in (PE, ACT, DVE, POOL) if self.last_real[e] is not None
                 and self.last_real[e].dsem is None]
        dl = [d.last for d in self.dsems if d.last is not None]
        for e in ENGS:
            op = Op(e, None, None)
            for d in lasts + dl:
                if d.dsem is None and d.eng == e and e == PE:
                    continue
                op.waits.append(d)
                if d.dsem is None:
                    d.inc = True
            self.ops[e].append(op)

    def emit(self):
        nc = self.nc
        for e in (PE, ACT, DVE, POOL):
            c = 0
            for op in self.ops[e]:
                if op.dsem is None and op.inc:
                    c += 1
                    op.cnt = c
        with nc.Block() as block:
            regs = {PE: block.tensor, ACT: block.scalar, DVE: block.vector, POOL: block.gpsimd, SP: block.sync}
            for e in ENGS:
                def body(engobj, e=e):
                    seen = {}
                    for op in self.ops[e]:
                        for d in op.waits:
                            if d.dsem is not None:
                                key, h, val = id(d.dsem), d.dsem.h, d.dval
                            else:
                                key, h, val = d.eng, self.esem[d.eng], d.cnt
                            if seen.get(key, 0) >= val:
                                continue
                            seen[key] = val
                            engobj.wait_ge(h, val)
                        if op.fn is None:
                            continue
                        ins = op.fn(engobj)
                        if op.dsem is not None:
                            ins.then_inc(op.dsem.h, 16)
                        elif op.inc:
                            ins.then_inc(self.esem[e], 1)
                regs[e](body)


class Arena:
    def __init__(self, base, words):
        self.base = base
        self.words = words
        self.off = 0

    def alloc(self, cols, dt):
        nb = 4 if dt == F32 else 2
        w = (cols * nb + 3) // 4
        assert self.off + w <= self.words, ("SBUF arena overflow", self.off, w, self.words)
        a = self.base[:, self.off:self.off + w]
        self.off += w
        return a if dt == F32 else a.bitcast(dt)


def tile_map(S):
    nt = S // 256
    g = [[0] * nt, [0] * nt]
    for m in range(nt):
        k, r = divmod(m, 2)
        g[0][m] = 4 * k + (0 if r == 0 else 3)
        g[1][m] = 4 * k + (1 if r == 0 else 2)
    return g


C_ID, C_TRI, C_OMT, C_ONE, C_MS, C_MC, C_RT = range(7)


def make_consts():
    c = np.zeros((128, 7, 128), np.float32)
    i = np.arange(128)
    c[:, C_ID] = np.eye(128, dtype=np.float32)
    c[:, C_TRI] = (i[:, None] >= i[None, :])
    c[:, C_OMT] = (i[:, None] < i[None, :])
    c[:, C_ONE] = 1.0
    c[:, C_MS] = (i[:, None] < i[None, :])
    c[:, C_MC] = (i[:, None] <= i[None, :])
    rt = np.zeros((128, 128), np.float32)
    for base in (0, 64):
        for d in range(8):
            rt[base + d + 8, base + d] = -1.0
            rt[base + d, base + d + 8] = 1.0
    c[:, C_RT] = rt
    return c.reshape(128, 7 * 128)


def rope_tables(pos):
    inv = (ROPE_THETA ** (-np.arange(0, ROPE_DIM, 2, dtype=np.float32) / ROPE_DIM)).astype(np.float32)
    ang = pos.astype(np.float32)[:, None] * inv[None, :]
    ang = np.concatenate([ang, ang], axis=-1)
    cos = np.ones((64, len(pos)), np.float32)
    sin = np.zeros((64, len(pos)), np.float32)
    cos[:ROPE_DIM] = np.cos(ang).T
    sin[:ROPE_DIM] = np.sin(ang).T
    return np.stack([np.concatenate([cos, cos], 0), np.concatenate([sin, sin], 0)], 0)


class Builder:
    def __init__(self, S, debug=(), stop_after=None):
        self.S = S
        self.NTS = S // 128
        self.NT = self.NTS // 2
        self.TOK = self.NT * 128
        self.debug = set(debug)
        self.stop_after = stop_after
        self.nc = bass.Bass("TRN2", target_bir_lowering=False)
        self.resd = {}

    def din(self, name, shape, dt=F32):
        return self.nc.dram_tensor(name, list(shape), dt, kind="ExternalInput").ap()

    def dscr(self, name, shape, dt):
        kind = "ExternalOutput" if name in self.debug else "Internal"
        return self.nc.dram_tensor(name, list(shape), dt, kind=kind).ap()

    def R(self, *key, track=True):
        r = self.resd.get(key)
        if r is None:
            r = Res(key, track)
            self.resd[key] = r
        return r

    def newR(self, name, track=True):
        return Res(name, track)

    def mm(self, out, lhsT, rhs, start, stop, reads, writes):
        return self.P.add(PE, lambda e: e.matmul(out, lhsT, rhs, start=start, stop=stop), reads, writes)

    def tr(self, out, in_, ident, reads, writes):
        return self.P.add(PE, lambda e: e.transpose(out, in_, ident), reads, writes)

    def act(self, out, in_, func, reads, writes, bias=None, scale=None, accum=None):
        kw = {}
        if bias is not None:
            kw["bias"] = bias
        if scale is not None:
            kw["scale"] = scale
        if accum is not None:
            kw["accum_out"] = accum
        return self.P.add(ACT, lambda e: e.activation(out=out, in_=in_, func=func, **kw), reads, writes)

    def tt(self, out, a, b, op, reads, writes, eng=DVE):
        return self.P.add(eng, lambda e: e.tensor_tensor(out=out, in0=a, in1=b, op=op), reads, writes)

    def ts(self, out, a, s1, s2, op0, op1, reads, writes, eng=DVE):
        if op1 is None:
            return self.P.add(eng, lambda e: e.tensor_scalar(out=out, in0=a, scalar1=s1, scalar2=None, op0=op0),
                              reads, writes)
        return self.P.add(eng, lambda e: e.tensor_scalar(out=out, in0=a, scalar1=s1, scalar2=s2, op0=op0, op1=op1),
                          reads, writes)

    def stt(self, out, a, scalar, b, op0, op1, reads, writes):
        return self.P.add(DVE, lambda e: e.scalar_tensor_tensor(out=out, in0=a, scalar=scalar, in1=b, op0=op0, op1=op1),
                          reads, writes)

    def cp(self, out, in_, reads, writes, eng=DVE):
        return self.P.add(eng, lambda e: e.tensor_copy(out=out, in_=in_), reads, writes)

    def dma(self, q, out, in_, dsem, reads, writes):
        return self.P.add(q, lambda e: e.dma_start(out=out, in_=in_), reads, writes, dsem="auto")

    def build(self):
        nc = self.nc
        S, NTS, NT, TOK = self.S, self.NTS, self.NT, self.TOK
        I = {}
        I["x"] = self.din("x", [S, D])
        I["cst"] = self.din("cst", [128, 7 * 128])
        I["ropek"] = self.din("ropek", [2, 128, S])
        I["ropeq"] = self.din("ropeq", [2, 128, TOK])
        I["sel"] = self.din("sel", [128, 2])
        I["mask1"] = self.din("mask1", [NT, 2, 128, 128])
        I["sb_norm_g"] = self.din("sb_norm_g", [D])
        I["sb_w_qkv"] = self.din("sb_w_qkv", [D, 3 * D])
        I["sb_w_o"] = self.din("sb_w_o", [D, D])
        I["kv_norm_g"] = self.din("kv_norm_g", [D])
        I["diff_w_kv"] = self.din("diff_w_kv", [D, 2 * D])
        I["diff_norm_g"] = self.din("diff_norm_g", [D])
        I["diff_w_q"] = self.din("diff_w_q", [D, D])
        I["lam"] = self.din("lam", [4, 64])
        I["diff_subln_g"] = self.din("diff_subln_g", [128, 1])
        I["diff_w_o"] = self.din("diff_w_o", [D, D])
        I["ffn_norm_g"] = self.din("ffn_norm_g", [2, D])
        I["dense_w_gate_up"] = self.din("dense_w_gate_up", [D, 2 * F_DENSE])
        I["dense_w_down"] = self.din("dense_w_down", [F_DENSE, D])
        I["moe_w_router"] = self.din("moe_w_router", [D, N_EXP])
        if self.stop_after in (None, "G"):
            I["moe_w_gate_up"] = self.din("moe_w_gate_up", [N_EXP, D, 2 * F_EXP])
            I["moe_w_down"] = self.din("moe_w_down", [N_EXP, F_EXP, D])
        I["final_norm_g"] = self.din("final_norm_g", [D])
        self.I = I
        out = nc.dram_tensor("out", [TOK, D], F32, kind="ExternalOutput").ap()
        X = {}
        X["q0T"] = self.dscr("q0T", [D, S], BF16)
        X["k0T"] = self.dscr("k0T", [D, S], BF16)
        X["v0"] = self.dscr("v0", [S, D], BF16)
        X["a0T"] = self.dscr("a0T", [D, S], BF16)
        X["h1"] = self.dscr("h1", [S, D], F32)
        X["k1T"] = self.dscr("k1T", [D, S], BF16)
        X["v1"] = self.dscr("v1", [S, D], BF16)
        X["hown"] = self.dscr("hown", [TOK, D], F32)
        X["q1T"] = self.dscr("q1T", [D, TOK], BF16)
        X["a1T"] = self.dscr("a1T", [D, TOK], BF16)
        self.X = X

        with contextlib.ExitStack() as es:
            self.P = Prog(nc, es)
            arena_words = 51 * 1024
            arena_t = es.enter_context(nc.sbuf_tensor("arena", [128, arena_words], F32))
            self.arena = Arena(arena_t[:, :], arena_words)
            ps_t = es.enter_context(nc.psum_tensor("ps", [128, 8, 512], F32))
            self.ps = ps_t
            self.psR = [self.newR(("psum", b)) for b in range(8)]
            self.sem_ld = [None] * 12
            self.sem_w = [None] * 8
            self.sem_st = [None] * 6
            self.sem_c = None
            self.sem_cp = None
            self.setup_consts()
            self.const_mark = self.arena.off

            stages = [
                ("A", self.stage_A), ("B", self.stage_B), ("C", self.stage_C), ("D", self.stage_D),
                ("E", self.stage_E), ("F", self.stage_F), ("G", lambda: self.stage_G(out)),
            ]
            for name, fn in stages:
                self.arena.off = self.const_mark
                fn()
                self.P.barrier()
                if self.stop_after == name:
                    break
            self.P.barrier()
            self.P.emit()
        return nc

    def setup_consts(self):
        A = self.arena
        cb = A.alloc(7 * 128, BF16).rearrange("p (a b) -> p a b", a=7)
        cf = A.alloc(7 * 128, F32).rearrange("p (a b) -> p a b", a=7)
        self.cR = self.newR("consts", track=False)
        src = self.I["cst"].rearrange("p (a b) -> p a b", a=7)
        self.dma(POOL, cb, src, self.sem_cp, [], [self.cR])
        self.dma(SP, cf, src, self.sem_c, [], [self.cR])
        self.cb, self.cf = cb, cf
        self.sel_sb = A.alloc(2, F32)
        self.eps_t = A.alloc(1, F32)
        self.P.add(DVE, lambda e: e.memset(self.eps_t, EPS), [], [self.cR])
        self.dma(SP, self.sel_sb, self.I["sel"], self.sem_c, [], [self.cR])

    def gain_bcast(self, gap, q=SP):
        t = self.arena.alloc(D, F32)
        r = self.newR("gain", track=False)
        self.dma(q, t, gap.unsqueeze(0).broadcast_to([128, D]), self.sem_c, [], [r])
        return t, r

    def rmsnorm_tile(self, src, srcR, gB, gR, out, outR, junk, junkR, stat, statR):
        self.P.add(DVE, lambda e: e.memset(stat[:, 0:1], 0.0), [], [statR])
        self.act(junk, src, AF.Square, [srcR, statR], [junkR, statR], accum=stat[:, 0:1])
        self.act(stat[:, 1:2], stat[:, 0:1], AF.Sqrt, [statR, self.cR], [statR], bias=self.eps_t, scale=1.0 / D)
        self.P.add(DVE, lambda e: e.reciprocal(out=stat[:, 1:2], in_=stat[:, 1:2]), [statR], [statR])
        self.stt(out, src, stat[:, 1:2], gB, ALU.mult, ALU.mult, [srcR, statR, gR], [outR])

    def proj_phase(self, tag, ntiles, src_fn, gain_ap, W_ap, ncols, fm_outs, tm_outs):
        A, P = self.arena, self.P
        cR = self.cR
        W_sb = A.alloc(8 * ncols, BF16).rearrange("p (k n) -> p k n", k=8)
        WR = self.newR(tag + "W", track=False)
        Wv = W_ap.rearrange("(k p) n -> p k n", p=128)
        for k in range(8):
            self.dma(POOL, W_sb[:, k, :], Wv[:, k, :], self.sem_w[k % 4], [], [WR])
        gB, gR = self.gain_bcast(gain_ap)
        GT = min(4, ntiles)
        NG = ntiles // GT
        GW = GT * 128
        xs = [A.alloc(D, F32) for _ in range(3)]
        xsR = [self.newR(tag + "xs%d" % i) for i in range(3)]
        xs2 = A.alloc(D, F32)
        xs2R = self.newR(tag + "xs2")
        xn = [A.alloc(D, BF16) for _ in range(2)]
        xnR = [self.newR(tag + "xn%d" % i) for i in range(2)]
        junk = A.alloc(D, BF16)
        junkR = self.newR(tag + "junk")
        stat = [A.alloc(2, F32) for _ in range(2)]
        statR = [self.newR(tag + "stat%d" % i) for i in range(2)]
        xnT = [A.alloc(8 * GW, BF16).rearrange("p (k t) -> p k t", k=8) for _ in range(2)]
        xnTR = [self.newR(tag + "xnT%d" % i) for i in range(2)]
        stg = [A.alloc(512, BF16) for _ in range(3)]
        stgR = [self.newR(tag + "stg%d" % i) for i in range(3)]
        need_rope = any(o.get("rope") is not None for o in fm_outs)
        if need_rope:
            qf = [A.alloc(GW, F32) for _ in range(2)]
            qfR = [self.newR(tag + "qf%d" % i) for i in range(2)]
            t1 = A.alloc(GW, F32)
            t1R = self.newR(tag + "t1")
            rt = [A.alloc(2 * GW, F32).rearrange("p (a t) -> p a t", a=2) for _ in range(2)]
            rtR = [self.newR(tag + "rt%d" % i) for i in range(2)]
        ps = self.ps
        n_ld = 0
        n_acc = 0
        n_stg = 0
        n_tr = 0
        n_qf = 0
        for g in range(NG):
            xT = xnT[g % 2]
            xTR = xnTR[g % 2]
            for tl in range(GT):
                ti = g * GT + tl
                srcs = src_fn(ti)
                s0 = xs[n_ld % 3]
                s0R = xsR[n_ld % 3]
                ld = self.sem_ld[n_ld % 3]
                n_ld += 1
                self.dma(SP, s0, srcs[0][0], ld, [], [s0R])
                if len(srcs) == 2:
                    self.dma(SP, xs2, srcs[1][0], self.sem_ld[3], [], [xs2R])
                    self.ts(s0, s0, self.sel_sb[:, 0:1], None, ALU.mult, None, [s0R, cR], [s0R])
                    self.stt(s0, xs2, self.sel_sb[:, 1:2], s0, ALU.mult, ALU.add, [xs2R, s0R, cR], [s0R])
                    if srcs[0][1] is not None:
                        self.dma(SP, srcs[0][1], s0, self.sem_st[0], [s0R], [self.R("hown", ti)])
                xo = xn[ti % 2]
                xoR = xnR[ti % 2]
                self.rmsnorm_tile(s0, s0R, gB, gR, xo, xoR, junk, junkR, stat[ti % 2], statR[ti % 2])
                bank = 6 + (n_tr % 2)
                n_tr += 1
                pb = ps[:, bank, :].bitcast(BF16).rearrange("p (k t) -> p k t", k=8)
                for k in range(8):
                    self.tr(pb[:, k, :], xo[:, k * 128:(k + 1) * 128], self.cb[:, C_ID, :], [xoR, cR], [self.psR[bank]])
                self.P.add(ACT, lambda e, o=xT[:, :, tl * 128:(tl + 1) * 128], i=pb: e.copy(out=o, in_=i),
                           [self.psR[bank]], [xTR])
            for o in fm_outs:
                if o.get("rope") is not None:
                    r_t = rt[g % 2]
                    r_R = rtR[g % 2]
                    self.dma(SP, r_t, o["rope"][:, :, g * GW:(g + 1) * GW].rearrange("a p t -> p a t"),
                             self.sem_ld[4 + g % 2], [], [r_R])
                for fo in range(8):
                    bank = n_acc % 3
                    n_acc += 1
                    pa = ps[:, bank, 0:GW]
                    for k in range(8):
                        self.mm(pa, W_sb[:, k, o["col0"] + fo * 128:o["col0"] + (fo + 1) * 128], xT[:, k, :],
                                k == 0, k == 7, [WR, xTR], [self.psR[bank]])
                    sg = stg[n_stg % 3][:, 0:GW]
                    sgR = stgR[n_stg % 3]
                    n_stg += 1
                    if o.get("rope") is None:
                        self.act(sg, pa, AF.Copy, [self.psR[bank]], [sgR], scale=float(o.get("scale", 1.0)))
                    else:
                        qq = qf[n_qf % 2]
                        qqR = qfR[n_qf % 2]
                        n_qf += 1
                        self.act(qq, pa, AF.Copy, [self.psR[bank]], [qqR], scale=float(o.get("scale", 1.0)))
                        rb = 3 + (n_qf % 2)
                        pr = ps[:, rb, 0:GW]
                        self.mm(pr, self.cf[:, C_RT, :], qq, True, True, [cR, qqR], [self.psR[rb]])
                        self.tt(t1, qq, r_t[:, 0, :], ALU.mult, [qqR, r_R], [t1R])
                        self.tt(qq, pr, r_t[:, 1, :], ALU.mult, [self.psR[rb], r_R], [qqR])
                        self.tt(sg, t1, qq, ALU.add, [t1R, qqR], [sgR])
                    self.dma(POOL, o["dst"][fo * 128:(fo + 1) * 128, g * GW:(g + 1) * GW], sg,
                             self.sem_st[1], [sgR], [self.R(o["name"], "g", g)])
            for o in tm_outs:
                for tl in range(GT):
                    ti = g * GT + tl
                    for hf in range(2):
                        bank = n_acc % 3
                        n_acc += 1
                        pa = ps[:, bank, :]
                        for k in range(8):
                            self.mm(pa, xT[:, k, tl * 128:(tl + 1) * 128],
                                    W_sb[:, k, o["col0"] + hf * 512:o["col0"] + (hf + 1) * 512],
                                    k == 0, k == 7, [WR, xTR], [self.psR[bank]])
                        sg = stg[n_stg % 3]
                        sgR = stgR[n_stg % 3]
                        n_stg += 1
                        self.cp(sg, pa, [self.psR[bank]], [sgR])
                        self.dma(POOL, o["dst"][ti * 128:(ti + 1) * 128, hf * 512:(hf + 1) * 512], sg,
                                 self.sem_st[2], [sgR], [self.R(o["name"], "t", ti)])

    def stage_A(self):
        I, X = self.I, self.X
        self.proj_phase(
            "A", self.NTS, lambda ti: [(I["x"][ti * 128:(ti + 1) * 128, :], None)], I["sb_norm_g"],
            I["sb_w_qkv"], 3 * D,
            fm_outs=[dict(name="q0T", col0=0, scale=0.125, dst=X["q0T"]),
                     dict(name="k0T", col0=D, dst=X["k0T"])],
            tm_outs=[dict(name="v0", col0=2 * D, dst=X["v0"])])

    def stage_B(self):
        A, P, ps, cR = self.arena, self.P, self.ps, self.cR
        S, NTS = self.S, self.NTS
        X = self.X
        cb = self.cb
        kT2 = [A.alloc(2 * S, BF16).rearrange("p (c t) -> p c t", c=2) for _ in range(2)]
        qT2 = [A.alloc(2 * NTS * 256, BF16).rearrange("p (c t w) -> p c t w", c=2, t=NTS) for _ in range(2)]
        vv2 = [A.alloc(NTS * 256, BF16).rearrange("p (b d) -> p b d", b=NTS) for _ in range(2)]
        kR2 = [self.newR("B_k%d" % i) for i in range(2)]
        qR2 = [self.newR("B_q%d" % i) for i in range(2)]
        vR2 = [self.newR("B_v%d" % i) for i in range(2)]
        for i_ in range(2):
            self.P.add(DVE, lambda e, t_=qT2[i_]: e.memset(t_, 0.0), [], [qR2[i_]])
        NB = 5
        e_sb = [A.alloc(512, F32) for _ in range(NB)]
        eR = [self.newR("B_e%d" % i) for i in range(NB)]
        sp_sb = [A.alloc(512, BF16) for _ in range(NB)]
        spR = [self.newR("B_sp%d" % i) for i in range(NB)]
        x_sb = [A.alloc(512, F32) for _ in range(2)]
        xR = [self.newR("B_x%d" % i) for i in range(2)]
        w_sb = [A.alloc(512, BF16) for _ in range(2)]
        wR = [self.newR("B_w%d" % i) for i in range(2)]
        o_sb = [A.alloc(512, BF16) for _ in range(2)]
        oR = [self.newR("B_o%d" % i) for i in range(2)]
        ss_sb = [A.alloc(512, BF16) for _ in range(2)]
        ssR = [self.newR("B_ss%d" % i) for i in range(2)]
        maskS4 = cb[:, C_MS, :].unsqueeze(1).broadcast_to([128, 4, 128])
        all_src = [self.R("q0T", "g", g) for g in range(max(1, NTS // 4))] + \
                  [self.R("k0T", "g", g) for g in range(max(1, NTS // 4))] + \
                  [self.R("v0", "t", t) for t in range(NTS)]
        gbase = 0
        for hg in range(4):
            kT, qT, vv = kT2[hg % 2], qT2[hg % 2], vv2[hg % 2]
            kR, qR, vR = kR2[hg % 2], qR2[hg % 2], vR2[hg % 2]
            for c in range(2):
                self.dma(SP, kT[:, c, :], X["k0T"][(hg * 2 + c) * 128:(hg * 2 + c + 1) * 128, :], None,
                         all_src, [kR])
                r0 = (hg * 2 + c) * 128
                for t0 in range(0, NTS, 8):
                    t1 = min(NTS, t0 + 8)
                    self.dma(SP, qT[0:64, c, t0:t1, 0:128],
                             X["q0T"][r0:r0 + 64, t0 * 128:t1 * 128].rearrange("p (t w) -> p t w", w=128),
                             None, all_src, [qR])
                    self.dma(SP, qT[64:128, c, t0:t1, 128:256],
                             X["q0T"][r0 + 64:r0 + 128, t0 * 128:t1 * 128].rearrange("p (t w) -> p t w", w=128),
                             None, all_src, [qR])
            for b0 in range(0, NTS, 4):
                b1 = min(NTS, b0 + 4)
                self.dma(SP, vv[:, b0:b1, :],
                         X["v0"][b0 * 128:b1 * 128, hg * 256:(hg + 1) * 256].rearrange("(b p) d -> p b d", p=128),
                         None, all_src, [vR])
            steps = [(qi, kb, i) for qi in range(NTS) for i, kb in enumerate(range(qi, -1, -1))]
            N = len(steps)

            def S1(k):
                qi, kb, i = steps[k]
                g = gbase + k
                zb = g % 2
                z = ps[:, zb, :]
                for cc in range(2):
                    self.mm(z[:, cc * 256:(cc + 1) * 256], kT[:, cc, kb * 128:(kb + 1) * 128],
                            qT[:, cc, qi, :], True, True, [kR, qR], [self.psR[zb]])

            def S2a(k):
                g = gbase + k
                zb, b = g % 2, g % NB
                self.act(e_sb[b], ps[:, zb, :], AF.Exp, [self.psR[zb]], [eR[b]])

            def S2(k):
                qi, kb, i = steps[k]
                g = gbase + k
                zb, b = g % 2, g % NB
                self.act(sp_sb[b], e_sb[b], AF.Ln, [eR[b]], [spR[b]], bias=1.0)
                if i == 0:
                    self.tt(sp_sb[b].rearrange("p (h t) -> p h t", h=4),
                            sp_sb[b].rearrange("p (h t) -> p h t", h=4), maskS4, ALU.mult,
                            [spR[b], cR], [spR[b]])

            def S3(k):
                qi, kb, i = steps[k]
                g = gbase + k
                b = g % NB
                last = (kb == 0)
                pbank = 2 + (g % 2)
                Pp = ps[:, pbank, :]
                if i == 0:
                    self.mm(Pp, cb[:, C_TRI, :], sp_sb[b], True, True, [cR, spR[b]], [self.psR[pbank]])
                else:
                    if i == 1:
                        car, carR = sp_sb[(g - 1) % NB], spR[(g - 1) % NB]
                    else:
                        car, carR = ss_sb[i % 2], ssR[i % 2]
                    self.mm(Pp, cb[:, C_TRI, :], sp_sb[b], True, False, [cR, spR[b]], [self.psR[pbank]])
                    self.mm(Pp, cb[:, C_ONE, :], car, False, True, [cR, carR], [self.psR[pbank]])
                    if not last:
                        self.tt(ss_sb[(i + 1) % 2], car, sp_sb[b], ALU.add, [carR, spR[b]], [ssR[(i + 1) % 2]])

            def S4(k):
                g = gbase + k
                pbank = 2 + (g % 2)
                self.act(x_sb[g % 2], ps[:, pbank, :], AF.Exp, [self.psR[pbank]], [xR[g % 2]], scale=-1.0)

            def S5(k):
                qi, kb, i = steps[k]
                g = gbase + k
                b, b2 = g % NB, g % 2
                self.tt(w_sb[b2], e_sb[b], x_sb[b2], ALU.mult, [eR[b], xR[b2]], [wR[b2]])
                if i == 0:
                    self.tt(w_sb[b2].rearrange("p (h t) -> p h t", h=4),
                            w_sb[b2].rearrange("p (h t) -> p h t", h=4), maskS4, ALU.mult,
                            [wR[b2], cR], [wR[b2]])

            def S6(k):
                qi, kb, i = steps[k]
                g = gbase + k
                b2 = g % 2
                first, last = (i == 0), (kb == 0)
                ob0 = 4 + 2 * (qi % 2)
                for pr in range(2):
                    self.mm(ps[:, ob0 + pr, 0:256], vv[:, kb, pr * 128:(pr + 1) * 128],
                            w_sb[b2][:, pr * 256:(pr + 1) * 256], first, last, [vR, wR[b2]],
                            [self.psR[ob0 + pr]])
                if last:
                    ob = o_sb[qi % 2]
                    self.cp(ob.rearrange("p (r c) -> p r c", r=2), ps[:, ob0:ob0 + 2, 0:256],
                            [self.psR[ob0], self.psR[ob0 + 1]], [oR[qi % 2]])
                    for pr in range(2):
                        for hf in range(2):
                            r0_ = hg * 256 + pr * 128 + hf * 64
                            self.dma(POOL, X["a0T"][r0_:r0_ + 64, qi * 128:(qi + 1) * 128],
                                     ob[hf * 64:(hf + 1) * 64, pr * 256 + hf * 128:pr * 256 + (hf + 1) * 128], None,
                                     [oR[qi % 2]], [self.R("a0T", qi // 4)])

            stages = (S1, S2a, S2, S3, S4, S5, S6)
            for it in range(-6, N):
                for si, fn in enumerate(stages):
                    k = it + 6 - si
                    if 0 <= k < N:
                        fn(k)
            gbase += N

    def tail_phase(self, tag, ntiles, aT, aT_res, Wo_ap, resid_fn, resid_res, gain_ap, experts, router_ap,
                   final_gain_ap, dst, dst_name):
        A, P, ps, cR = self.arena, self.P, self.ps, self.cR
        cb, cf = self.cb, self.cf
        GT = min(4, ntiles)
        NG = ntiles // GT
        GW = GT * 128
        Wo = A.alloc(8 * D, BF16).rearrange("p (k n) -> p k n", k=8)
        WoR = self.newR(tag + "Wo", track=False)
        Wov = Wo_ap.rearrange("(k p) n -> p k n", p=128)
        for k in range(8):
            self.dma(POOL, Wo[:, k, :], Wov[:, k, :], self.sem_w[4 + k % 2], [], [WoR])
        gB, gR = self.gain_bcast(gain_ap)
        if final_gain_ap is not None:
            gF, gFR = self.gain_bcast(final_gain_ap)
        if router_ap is not None:
            wr = A.alloc(8 * N_EXP, F32).rearrange("p (k n) -> p k n", k=8)
            wrR = self.newR(tag + "wr", track=False)
            self.dma(SP, wr, router_ap.rearrange("(k p) n -> p k n", p=128), self.sem_c, [], [wrR])
            hnT32 = A.alloc(8 * 128, F32).rearrange("p (k t) -> p k t", k=8)
            hnT32R = self.newR(tag + "hnT32")
            comb = A.alloc(GT * N_EXP, F32).rearrange("p (t n) -> p t n", t=GT)
            combR = self.newR(tag + "comb")
            rw = A.alloc(64, F32)
            rwR = self.newR(tag + "rw")
        aT_sb = A.alloc(8 * GW, BF16).rearrange("p (k t) -> p k t", k=8)
        aTR = self.newR(tag + "aT")
        acc = A.alloc(GT * D, F32).rearrange("p (t n) -> p t n", t=GT)
        accR = [self.newR(tag + "acc%d" % i) for i in range(GT)]
        rs = [A.alloc(D, F32) for _ in range(2)]
        rsR = [self.newR(tag + "rs%d" % i) for i in range(2)]
        hn = A.alloc(D, F32 if router_ap is not None else BF16)
        hnR = self.newR(tag + "hn")
        junk = A.alloc(D, BF16)
        junkR = self.newR(tag + "junk")
        stat = A.alloc(2, F32)
        statR = self.newR(tag + "stat")
        hnT = A.alloc(8 * GW, BF16).rearrange("p (k t) -> p k t", k=8)
        hnTR = self.newR(tag + "hnT")
        NFmax = max(F for (_, _, F) in experts) // 128
        actT = A.alloc(NFmax * GW, BF16).rearrange("p (f t) -> p f t", f=NFmax)
        actR = self.newR(tag + "actT")
        sg = [A.alloc(GW, F32) for _ in range(2)]
        sgR = [self.newR(tag + "sg%d" % i) for i in range(2)]
        NWS = 3
        wgu = [A.alloc(8 * 2 * 512, BF16).rearrange("p (k a n) -> p k a n", k=8, a=2) for _ in range(NWS)]
        wguR = [self.newR(tag + "wgu%d" % i) for i in range(NWS)]
        NDS = 2
        NBDmax = max((7 if (F // 128) % 7 == 0 else 11) for (_, _, F) in experts)
        wd = [A.alloc(NBDmax * D, BF16).rearrange("p (c n) -> p c n", c=NBDmax) for _ in range(NDS)]
        wdR = [self.newR(tag + "wd%d" % i) for i in range(NDS)]
        ot, otR = rs, rsR
        n_acc = 0
        n_gu = 0
        n_wgu = 0
        n_wd = 0
        n_sg = 0
        for g in range(NG):
            self.dma(SP, aT_sb, aT[:, g * GW:(g + 1) * GW].rearrange("(k p) t -> p k t", p=128), self.sem_ld[6],
                     aT_res(g), [aTR])
            for tl in range(GT):
                ti = g * GT + tl
                r_t, r_R = rs[ti % 2], rsR[ti % 2]
                self.dma(SP, r_t, resid_fn(ti), self.sem_ld[7 + ti % 2], resid_res(ti), [r_R])
                for hf in range(2):
                    bank = n_acc % 2
                    n_acc += 1
                    pa = ps[:, bank, :]
                    for k in range(8):
                        self.mm(pa, aT_sb[:, k, tl * 128:(tl + 1) * 128], Wo[:, k, hf * 512:(hf + 1) * 512],
                                k == 0, k == 7, [aTR, WoR], [self.psR[bank]])
                    self.tt(acc[:, tl, hf * 512:(hf + 1) * 512], pa, r_t[:, hf * 512:(hf + 1) * 512], ALU.add,
                            [self.psR[bank], r_R], [accR[tl]])
                self.rmsnorm_tile(acc[:, tl, :], accR[tl], gB, gR, hn, hnR, junk, junkR, stat, statR)
                if router_ap is None:
                    bank = 6 + (ti % 2)
                    pb = ps[:, bank, :].bitcast(BF16).rearrange("p (k t) -> p k t", k=8)
                    for k in range(8):
                        self.tr(pb[:, k, :], hn[:, k * 128:(k + 1) * 128], cb[:, C_ID, :], [hnR, cR], [self.psR[bank]])
                    self.P.add(ACT, lambda e, o=hnT[:, :, tl * 128:(tl + 1) * 128], i=pb: e.copy(out=o, in_=i),
                               [self.psR[bank]], [hnTR])
                else:
                    p32 = ps[:, 6:8, :].rearrange("p b (k t) -> p (b k) t", k=4)
                    for k in range(8):
                        self.tr(p32[:, k, :], hn[:, k * 128:(k + 1) * 128], cf[:, C_ID, :], [hnR, cR],
                                [self.psR[6], self.psR[7]])
                    self.P.add(ACT, lambda e, o=hnT32, i=p32: e.copy(out=o, in_=i), [self.psR[6], self.psR[7]], [hnT32R])
                    self.cp(hnT[:, :, tl * 128:(tl + 1) * 128], hnT32, [hnT32R], [hnTR])
                    bank = n_acc % 2
                    n_acc += 1
                    pl = ps[:, bank, 0:N_EXP]
                    for k in range(8):
                        self.mm(pl, hnT32[:, k, :], wr[:, k, :], k == 0, k == 7, [hnT32R, wrR], [self.psR[bank]])
                    L, E1, L2, E2, SC, C1 = (rw[:, i * 8:(i + 1) * 8] for i in range(6))
                    rr = [rwR]
                    self.cp(L, pl, [self.psR[bank]], rr)
                    P.add(DVE, lambda e, o=SC[:, 0:1], i=L: e.tensor_reduce(out=o, in_=i, axis=AX.X, op=ALU.max), rr, rr)
                    self.ts(E1, L, SC[:, 0:1], None, ALU.is_equal, None, rr, rr)
                    self.stt(L2, E1, -1e30, L, ALU.mult, ALU.add, rr, rr)
                    P.add(DVE, lambda e, o=SC[:, 1:2], i=L2: e.tensor_reduce(out=o, in_=i, axis=AX.X, op=ALU.max), rr, rr)
                    self.ts(E2, L2, SC[:, 1:2], None, ALU.is_equal, None, rr, rr)
                    self.tt(SC[:, 2:3], SC[:, 1:2], SC[:, 0:1], ALU.subtract, rr, rr)
                    self.act(SC[:, 3:4], SC[:, 2:3], AF.Exp, rr, rr)
                    self.ts(SC[:, 4:5], SC[:, 3:4], 1.0, None, ALU.add, None, rr, rr)
                    P.add(DVE, lambda e, o=SC[:, 5:6], i=SC[:, 4:5]: e.reciprocal(out=o, in_=i), rr, rr)
                    self.tt(SC[:, 6:7], SC[:, 3:4], SC[:, 5:6], ALU.mult, rr, rr)
                    self.ts(C1, E1, SC[:, 5:6], None, ALU.mult, None, rr, rr)
                    self.stt(comb[:, tl, :], E2, SC[:, 6:7], C1, ALU.mult, ALU.add, rr, [combR])
            for ei, (Wgu_ap, Wd_ap, F) in enumerate(experts):
                NF = F // 128
                BF = 4 if NF % 4 == 0 else 2
                Wg_v = Wgu_ap.rearrange("(k p) n -> p k n", p=128)
                for blk in range(NF // BF):
                    s = n_wgu % NWS
                    n_wgu += 1
                    wsl, wslR = wgu[s], wguR[s]
                    bw = BF * 128
                    self.dma(POOL, wsl[:, :, 0, 0:bw], Wg_v[:, :, blk * bw:(blk + 1) * bw], self.sem_w[s], [], [wslR])
                    self.dma(POOL, wsl[:, :, 1, 0:bw], Wg_v[:, :, F + blk * bw:F + (blk + 1) * bw], self.sem_w[s], [],
                             [wslR])
                    for c in range(BF):
                        fc = blk * BF + c
                        gb = 2 + (n_gu % 2)
                        ub = 4 + (n_gu % 2)
                        n_gu += 1
                        pg = ps[:, gb, 0:GW]
                        pu = ps[:, ub, 0:GW]
                        for k in range(8):
                            self.mm(pg, wsl[:, k, 0, c * 128:(c + 1) * 128], hnT[:, k, :], k == 0, k == 7,
                                    [wslR, hnTR], [self.psR[gb]])
                        for k in range(8):
                            self.mm(pu, wsl[:, k, 1, c * 128:(c + 1) * 128], hnT[:, k, :], k == 0, k == 7,
                                    [wslR, hnTR], [self.psR[ub]])
                        s_t, s_R = sg[n_sg % 2], sgR[n_sg % 2]
                        n_sg += 1
                        self.act(s_t, pg, AF.Silu, [self.psR[gb]], [s_R])
                        self.tt(actT[:, fc, :], s_t, pu, ALU.mult, [s_R, self.psR[ub]], [actR])
                NBD = 7 if NF % 7 == 0 else 11
                Wd_v = Wd_ap.rearrange("(c p) n -> p c n", p=128)
                for blk in range(NF // NBD):
                    s = n_wd % NDS
                    n_wd += 1
                    dsl, dslR = wd[s], wdR[s]
                    self.dma(POOL, dsl[:, 0:NBD, :], Wd_v[:, blk * NBD:(blk + 1) * NBD, :], self.sem_w[4 + s], [], [dslR])
                    for tl in range(GT):
                        for hf in range(2):
                            bank = n_acc % 2
                            n_acc += 1
                            pa = ps[:, bank, :]
                            for c in range(NBD):
                                self.mm(pa, actT[:, blk * NBD + c, tl * 128:(tl + 1) * 128],
                                        dsl[:, c, hf * 512:(hf + 1) * 512], c == 0, c == NBD - 1,
                                        [actR, dslR], [self.psR[bank]])
                            av = acc[:, tl, hf * 512:(hf + 1) * 512]
                            if router_ap is None:
                                self.tt(av, pa, av, ALU.add, [self.psR[bank], accR[tl]], [accR[tl]])
                            else:
                                self.stt(av, pa, comb[:, tl, ei:ei + 1], av, ALU.mult, ALU.add,
                                         [self.psR[bank], accR[tl], combR], [accR[tl]])
            for tl in range(GT):
                ti = g * GT + tl
                if final_gain_ap is None:
                    self.dma(SP, dst[ti * 128:(ti + 1) * 128, :], acc[:, tl, :], self.sem_st[4], [accR[tl]],
                             [self.R(dst_name, ti)])
                else:
                    o_t, o_R = ot[ti % 2], otR[ti % 2]
                    self.rmsnorm_tile(acc[:, tl, :], accR[tl], gF, gFR, o_t, o_R, junk, junkR, stat, statR)
                    self.dma(SP, dst[ti * 128:(ti + 1) * 128, :], o_t, self.sem_st[4], [o_R], [self.R(dst_name, ti)])

    def stage_C(self):
        I, X = self.I, self.X
        self.tail_phase(
            "C", self.NTS, X["a0T"], lambda g: [self.R("a0T", g)], I["sb_w_o"],
            lambda ti: I["x"][ti * 128:(ti + 1) * 128, :], lambda ti: [],
            I["ffn_norm_g"][0], [(I["dense_w_gate_up"], I["dense_w_down"], F_DENSE)], None, None,
            X["h1"], "h1")

    def stage_D(self):
        I, X = self.I, self.X
        self.proj_phase(
            "D", self.NTS, lambda ti: [(X["h1"][ti * 128:(ti + 1) * 128, :], None)], I["kv_norm_g"],
            I["diff_w_kv"], 2 * D,
            fm_outs=[dict(name="k1T", col0=0, dst=X["k1T"], rope=I["ropek"])],
            tm_outs=[dict(name="v1", col0=D, dst=X["v1"])])

    def stage_E(self):
        I, X = self.I, self.X
        gm = tile_map(self.S)

        def src(m):
            a, b = gm[0][m], gm[1][m]
            return [(X["h1"][a * 128:(a + 1) * 128, :], X["hown"][m * 128:(m + 1) * 128, :]),
                    (X["h1"][b * 128:(b + 1) * 128, :], None)]

        self.proj_phase(
            "E", self.NT, src, I["diff_norm_g"], I["diff_w_q"], D,
            fm_outs=[dict(name="q1T", col0=0, dst=X["q1T"], rope=I["ropeq"], scale=0.125)], tm_outs=[])

    def stage_F(self):
        A, P, ps, cR = self.arena, self.P, self.ps, self.cR
        S, NTS, NT, TOK = self.S, self.NTS, self.NT, self.TOK
        I, X = self.I, self.X
        cb, cf = self.cb, self.cf
        lv = A.alloc(4 * 64, F32).rearrange("p (a d) -> p a d", a=4)
        lw = A.alloc(8, F32)
        lR = self.newR("F_lam")
        self.dma(SP, lv, I["lam"].unsqueeze(0).broadcast_to([128, 4, 64]), self.sem_c, [], [lR])
        pr = A.alloc(128, F32).rearrange("p (a d) -> p a d", a=2)
        self.tt(pr[:, 0, :], lv[:, 0, :], lv[:, 1, :], ALU.mult, [lR], [lR])
        self.tt(pr[:, 1, :], lv[:, 2, :], lv[:, 3, :], ALU.mult, [lR], [lR])
        P.add(DVE, lambda e: e.tensor_reduce(out=lw[:, 0:2], in_=pr, axis=AX.X, op=ALU.add), [lR], [lR])
        self.act(lw[:, 2:4], lw[:, 0:2], AF.Exp, [lR], [lR])
        lam_init = 0.8 - 0.6 * math.exp(-0.3 * 1)
        self.tt(lw[:, 4:5], lw[:, 3:4], lw[:, 2:3], ALU.subtract, [lR], [lR])
        self.ts(lw[:, 5:6], lw[:, 4:5], -float(lam_init), None, ALU.add, None, [lR], [lR])
        neg_lam = lw[:, 5:6]
        gs = A.alloc(2, F32)
        self.dma(SP, gs[:, 0:1], I["diff_subln_g"], self.sem_c, [], [lR])
        self.ts(gs[:, 1:2], gs[:, 0:1], float(1.0 - lam_init), None, ALU.mult, None, [lR], [lR])
        gsub = gs[:, 1:2]
        kT2 = [A.alloc(2 * S, BF16).rearrange("p (c t) -> p c t", c=2) for _ in range(2)]
        qT2 = [A.alloc(2 * NT * 256, BF16).rearrange("p (c t w) -> p c t w", c=2, t=NT) for _ in range(2)]
        vv2 = [A.alloc(NTS * 256, BF16).rearrange("p (b d) -> p b d", b=NTS) for _ in range(2)]
        kR2 = [self.newR("F_k%d" % i) for i in range(2)]
        qR2 = [self.newR("F_q%d" % i) for i in range(2)]
        vR2 = [self.newR("F_v%d" % i) for i in range(2)]
        for i_ in range(2):
            self.P.add(DVE, lambda e, t_=qT2[i_]: e.memset(t_, 0.0), [], [qR2[i_]])
        mk = A.alloc(NT * 2 * 128, BF16).rearrange("p (m a t) -> p m a t", m=NT, a=2)
        mkR = self.newR("F_mask", track=False)
        for m0 in range(0, NT, 2):
            m1 = min(NT, m0 + 2)
            self.dma(POOL, mk[:, m0:m1], I["mask1"][m0:m1].rearrange("m a s t -> s m a t"), None, [], [mkR])
        e_sb = [A.alloc(512, BF16) for _ in range(3)]
        eR = [self.newR("F_e%d" % i) for i in range(3)]
        f1 = A.alloc(512, F32)
        f2 = A.alloc(512, F32)
        f3 = A.alloc(256, F32)
        f4 = A.alloc(256, F32)
        fR = self.newR("F_fin")
        on = [A.alloc(256, BF16) for _ in range(2)]
        onR = [self.newR("F_on%d" % i) for i in range(2)]
        src_all = [self.R("k1T", "g", g) for g in range(max(1, NTS // 4))] + \
                  [self.R("v1", "t", t) for t in range(NTS)] + \
                  [self.R("q1T", "g", g) for g in range(max(1, NT // 4))]
        gbase = 0
        for hp in range(4):
            kT, qT, vv = kT2[hp % 2], qT2[hp % 2], vv2[hp % 2]
            kR, qR, vR = kR2[hp % 2], qR2[hp % 2], vR2[hp % 2]
            for c in range(2):
                h = hp * 2 + c
                self.dma(SP, kT[:, c, :], X["k1T"][h * 128:(h + 1) * 128, :], self.sem_ld[0], src_all, [kR])
                for t0 in range(0, NT, 8):
                    t1 = min(NT, t0 + 8)
                    self.dma(SP, qT[0:64, c, t0:t1, 0:128],
                             X["q1T"][h * 128:h * 128 + 64, t0 * 128:t1 * 128].rearrange("p (t w) -> p t w", w=128),
                             None, src_all, [qR])
                    self.dma(SP, qT[64:128, c, t0:t1, 128:256],
                             X["q1T"][h * 128 + 64:(h + 1) * 128, t0 * 128:t1 * 128].rearrange("p (t w) -> p t w", w=128),
                             None, src_all, [qR])
            for b0 in range(0, NTS, 4):
                b1 = min(NTS, b0 + 4)
                self.dma(SP, vv[:, b0:b1, :],
                         X["v1"][b0 * 128:b1 * 128, hp * 256:(hp + 1) * 256].rearrange("(b p) d -> p b d", p=128),
                         None, src_all, [vR])
            steps = []
            for m in range(NT):
                k4, r = divmod(m, 2)
                gmax = 4 * k4 + (1 if r == 0 else 3)
                steps += [(m, kb, gmax) for kb in range(gmax + 1)]
            N = len(steps)

            def S1(k):
                m, kb, gmax = steps[k]
                zb = (gbase + k) % 2
                z = ps[:, zb, :]
                for hh in range(2):
                    self.mm(z[:, hh * 256:(hh + 1) * 256], kT[:, hh, kb * 128:(kb + 1) * 128],
                            qT[:, hh, m, :], True, True, [kR, qR], [self.psR[zb]])

            def S2(k):
                m, kb, gmax = steps[k]
                g = gbase + k
                zb, eb = g % 2, g % 3
                self.act(e_sb[eb], ps[:, zb, :], AF.Exp, [self.psR[zb]], [eR[eb]])
                if kb >= gmax - 1:
                    mm_ = mk[:, m, kb - (gmax - 1), :].unsqueeze(1).broadcast_to([128, 4, 128])
                    ev = e_sb[eb].rearrange("p (h t) -> p h t", h=4)
                    self.tt(ev, ev, mm_, ALU.mult, [eR[eb], mkR], [eR[eb]])

            def S3(k):
                m, kb, gmax = steps[k]
                g = gbase + k
                eb = g % 3
                ob0 = 2 + 2 * (m % 2)
                sbank = 6 + (m % 2)
                Oa = ps[:, ob0:ob0 + 2, 0:256]
                Sa = ps[:, sbank, :]
                for hh in range(2):
                    self.mm(ps[:, ob0 + hh, 0:256], vv[:, kb, hh * 128:(hh + 1) * 128],
                            e_sb[eb][:, hh * 256:(hh + 1) * 256], kb == 0, kb == gmax, [vR, eR[eb]],
                            [self.psR[ob0 + hh]])
                self.mm(Sa, cb[:, C_ONE, :], e_sb[eb], kb == 0, kb == gmax, [cR, eR[eb]], [self.psR[sbank]])
                if kb != gmax:
                    return
                rr = [fR]
                self.act(f1, Sa, AF.Ln, [self.psR[sbank]], rr)
                self.act(f1, f1, AF.Exp, rr, rr, scale=-1.0)
                self.tt(f2.rearrange("p (h c) -> p h c", h=2), Oa, f1.rearrange("p (h c) -> p h c", h=2), ALU.mult,
                        [self.psR[ob0], self.psR[ob0 + 1]] + rr, rr)
                f2v = f2.rearrange("p (h a t) -> p h a t", h=2, a=2)
                f3v = f3.rearrange("p (h t) -> p h t", h=2)
                self.stt(f3v, f2v[:, :, 1, :], neg_lam, f2v[:, :, 0, :], ALU.mult, ALU.add, rr + [lR], rr)
                self.tt(f4, f3, f3, ALU.mult, rr, rr)
                pm = ps[:, sbank, 0:256]
                self.mm(pm, cf[:, C_ONE, :], f4, True, True, [cR] + rr, [self.psR[sbank]])
                self.act(f4, pm, AF.Ln, [self.psR[sbank], cR], rr, bias=self.eps_t, scale=1.0 / 128.0)
                self.act(f4, f4, AF.Exp, rr, rr, scale=-0.5)
                self.tt(f3, f3, f4, ALU.mult, rr, rr)
                o_t, o_R = on[m % 2], onR[m % 2]
                self.ts(o_t, f3, gsub, None, ALU.mult, None, rr + [lR], [o_R])
                dst = X["a1T"][hp * 256:(hp + 1) * 256, m * 128:(m + 1) * 128].rearrange("(h d) t -> d h t", d=128)
                self.dma(POOL, dst, o_t.rearrange("p (h t) -> p h t", h=2), None, [o_R], [self.R("a1T", m // 4)])

            for it in range(-2, N):
                for si, fn in enumerate((S1, S2, S3)):
                    k = it + 2 - si
                    if 0 <= k < N:
                        fn(k)
            gbase += N

    def stage_G(self, out):
        I, X = self.I, self.X
        experts = [(I["moe_w_gate_up"][e], I["moe_w_down"][e], F_EXP) for e in range(N_EXP)]
        self.tail_phase(
            "G", self.NT, X["a1T"], lambda g: [self.R("a1T", g)], I["diff_w_o"],
            lambda ti: X["hown"][ti * 128:(ti + 1) * 128, :], lambda ti: [self.R("hown", ti)],
            I["ffn_norm_g"][1], experts, I["moe_w_router"], I["final_norm_g"], out, "out")


def make_in_maps(inputs, S):
    gm = tile_map(S)
    NT = S // 256
    cst = make_consts()
    ropek = rope_tables(np.arange(S))
    i = np.arange(128)
    ones = np.ones((128, 128), np.float32)
    zeros = np.zeros((128, 128), np.float32)
    diag = (i[:, None] <= i[None, :]).astype(np.float32)
    f = lambda a: np.ascontiguousarray(np.asarray(a, dtype=np.float32))
    shared = {
        "cst": cst, "ropek": ropek,
        "sb_norm_g": f(inputs["sb_norm_g"][0]), "sb_w_qkv": f(inputs["sb_w_qkv"][0]), "sb_w_o": f(inputs["sb_w_o"][0]),
        "kv_norm_g": f(inputs["kv_norm_g"]), "diff_w_kv": f(inputs["diff_w_kv"]),
        "diff_norm_g": f(inputs["diff_norm_g"][0]), "diff_w_q": f(inputs["diff_w_q"][0]),
        "lam": f(np.stack([inputs["diff_lambda_q1"][0], inputs["diff_lambda_k1"][0],
                           inputs["diff_lambda_q2"][0], inputs["diff_lambda_k2"][0]], 0)),
        "diff_subln_g": f(inputs["diff_subln_g"][0]).reshape(128, 1),
        "diff_w_o": f(inputs["diff_w_o"][0]), "ffn_norm_g": f(inputs["ffn_norm_g"]),
        "dense_w_gate_up": f(inputs["dense_w_gate_up"][0]), "dense_w_down": f(inputs["dense_w_down"][0]),
        "moe_w_router": f(inputs["moe_w_router"][0]), "moe_w_gate_up": f(inputs["moe_w_gate_up"][0]),
        "moe_w_down": f(inputs["moe_w_down"][0]), "final_norm_g": f(inputs["final_norm_g"]),
    }
    x = np.asarray(inputs["x"], dtype=np.float32)
    maps = []
    for c in range(8):
        b, j = divmod(c, 2)
        pos = np.concatenate([gm[j][m] * 128 + np.arange(128) for m in range(NT)])
        mask1 = np.zeros((NT, 2, 128, 128), np.float32)
        for m in range(NT):
            k4, r = divmod(m, 2)
            gmax = 4 * k4 + (1 if r == 0 else 3)
            g = gm[j][m]
            for a, kb in enumerate((gmax - 1, gmax)):
                mask1[m, a] = ones if kb < g else (diag if kb == g else zeros)
        sel = np.zeros((128, 2), np.float32)
        sel[:, j] = 1.0
        d = dict(shared)
        d.update({"x": np.ascontiguousarray(x[b]), "ropeq": rope_tables(pos), "sel": sel, "mask1": mask1})
        maps.append(d)
    return maps


_NC_CACHE = {}


def kernel(**inputs):
    S = int(np.asarray(inputs["x"]).shape[1])
    B = int(np.asarray(inputs["x"]).shape[0])
    assert B == 4
    if S not in _NC_CACHE:
        _NC_CACHE[S] = Builder(S).build()
    nc = _NC_CACHE[S]
    maps = make_in_maps(inputs, S)
    res = run_bass_kernel_spmd(nc, maps, core_ids=list(range(8)))
    gm = tile_map(S)
    NT = S // 256
    out = np.zeros((B, S, D), np.float32)
    for c in range(8):
        b, j = divmod(c, 2)
        o = np.asarray(res.results[c]["out"]).reshape(NT, 128, D)
        for m in range(NT):
            g = gm[j][m]
            out[b, g * 128:(g + 1) * 128] = o[m]
    return out
```
